# Optimizing a Trainium2 kernel written in Bass

```python
import jax
import jax.numpy as jnp
from jax import lax
import numpy as np


D_MODEL = 1024
BATCH = 4
SEQ = 8192
DEPTH = 1

GRID_W = 64
CTX_LEN = 256
CONV_CH = 512
CONV_ROW_CH = CONV_CH // 2
CONV_WIDTH = 31
LRU_WIDTH = 512
LRU_HEADS = 8
LRU_HEAD_DIM = LRU_WIDTH // LRU_HEADS
LRU_CONV_WIDTH = 4
LRU_C = 8.0
N_DIRS = 2
D_IN = 2 * CONV_CH + 2 * LRU_WIDTH
D_MIX = CONV_CH + LRU_WIDTH
N_EXPERTS = 16
EC_CAPACITY = 2
D_EXPERT = 2816
N_MOD = 6
RMS_EPS = 1e-6
LN_EPS = 1e-5

kernel_name = "hybrid_conformer_rglru_ecmoe_dit"


def rms_norm(x, g):
    xf = x.astype(jnp.float32)
    y = xf * lax.rsqrt(jnp.mean(xf * xf, axis=-1, keepdims=True) + RMS_EPS)
    return (y * g.astype(jnp.float32)).astype(x.dtype)


def layer_norm(x, g, b):
    xf = x.astype(jnp.float32)
    mu = jnp.mean(xf, axis=-1, keepdims=True)
    var = jnp.mean(jnp.square(xf - mu), axis=-1, keepdims=True)
    y = (xf - mu) * lax.rsqrt(var + LN_EPS) * g.astype(jnp.float32) + b.astype(jnp.float32)
    return y.astype(x.dtype)


def adaln(cond, w, b):
    return jnp.split(jax.nn.silu(cond) @ w + b, N_MOD, axis=-1)


def modulate(h, shift, scale):
    return h * (1.0 + scale[:, None, :]) + shift[:, None, :]


def depthwise_conv_seq(u, w, pad):
    return lax.conv_general_dilated(u, w[:, None, :].astype(u.dtype), window_strides=(1,), padding=[pad],
                                    dimension_numbers=('NWC', 'WIO', 'NWC'), feature_group_count=u.shape[-1])


def depthwise_conv_grid(u, w):
    bsz, n, ch = u.shape
    rows = n // GRID_W
    pad = (CONV_WIDTH - 1) // 2
    g = u.reshape(bsz, rows, GRID_W, ch)
    w = w.astype(u.dtype)
    dn = ('NHWC', 'HWIO', 'NHWC')
    n_col = ch - CONV_ROW_CH
    y_row = lax.conv_general_dilated(g[..., :CONV_ROW_CH], w[:, :CONV_ROW_CH].reshape(1, CONV_WIDTH, 1, CONV_ROW_CH),
                                     (1, 1), [(0, 0), (pad, pad)], dimension_numbers=dn, feature_group_count=CONV_ROW_CH)
    y_col = lax.conv_general_dilated(g[..., CONV_ROW_CH:], w[:, CONV_ROW_CH:].reshape(CONV_WIDTH, 1, 1, n_col),
                                     (1, 1), [(pad, pad), (0, 0)], dimension_numbers=dn, feature_group_count=n_col)
    return jnp.concatenate([y_row, y_col], axis=-1).reshape(bsz, n, ch)


def conformer_tail(y, g, b):
    return jax.nn.silu(layer_norm(y, g, b))


def mixer_inputs(h, norm_g, shift, scale, w_in, b_in):
    u = modulate(rms_norm(h, norm_g), shift, scale)
    p = u @ w_in + b_in
    cv, cg, lx, lg = jnp.split(p, [CONV_CH, 2 * CONV_CH, 2 * CONV_CH + LRU_WIDTH], axis=-1)
    return cv * jax.nn.sigmoid(cg), lx, lg


def _linear_combine(left, right):
    a1, b1 = left
    a2, b2 = right
    return a1 * a2, a2 * b1 + b2


def rg_lru_dir(xr, lru_p, d, h0):
    conv_w, conv_b, wa, ba, wi, bi, lam = lru_p
    bsz, n, ch = xr.shape
    xc = depthwise_conv_seq(xr, conv_w[d], (LRU_CONV_WIDTH - 1, 0)) + conv_b[d]
    xh = xc.reshape(bsz, n, LRU_HEADS, LRU_HEAD_DIM)
    r = jax.nn.sigmoid((jnp.einsum('bnhi,hij->bnhj', xh, wa[d]).reshape(bsz, n, ch) + ba[d]).astype(jnp.float32))
    i = jax.nn.sigmoid((jnp.einsum('bnhi,hij->bnhj', xh, wi[d]).reshape(bsz, n, ch) + bi[d]).astype(jnp.float32))
    log_a = -LRU_C * r * jax.nn.softplus(-lam[d].astype(jnp.float32))
    a = jnp.exp(log_a)
    u = jnp.sqrt(-jnp.expm1(2.0 * log_a)) * (i * xc.astype(jnp.float32))
    a_cum, u_cum = lax.associative_scan(_linear_combine, (a, u), axis=1)
    return a_cum * h0[:, None, :] + u_cum


def merge_heads(conv_y, h_sum, lg, w_out, b_out):
    y_lru = (h_sum * jax.nn.gelu(lg.astype(jnp.float32))).astype(conv_y.dtype)
    return jnp.concatenate([conv_y, y_lru], axis=-1) @ w_out + b_out


def ec_moe(v, router_w, wg, wu, wd):
    bsz, n, _ = v.shape
    cap = EC_CAPACITY * n // N_EXPERTS
    aff = jax.nn.softmax(jnp.einsum('bnd,de->bne', v, router_w).astype(jnp.float32), axis=-1)
    gate, idx = lax.top_k(jnp.swapaxes(aff, 1, 2), cap)
    bidx = jnp.arange(bsz)[:, None, None]
    xs = jnp.swapaxes(v[bidx, idx], 0, 1)

    def expert(args):
        xe, wge, wue, wde = args
        return (jax.nn.silu(xe @ wge) * (xe @ wue)) @ wde

    ys = jnp.swapaxes(lax.map(expert, (xs, wg, wu, wd)), 0, 1)
    ys = ys * gate[..., None].astype(v.dtype)
    return jnp.zeros_like(v).at[bidx, idx].add(ys)


def hybrid_layer(x, ctx, c, c_ctx, norm1_g, norm2_g, ada_w, ada_b, w_in, b_in, conv_p, lru_p,
                 w_out, b_out, moe_p, update_ctx):
    conv_w, conv_b, ln_g, ln_b = conv_p
    mod = adaln(c, ada_w, ada_b)
    mod_c = adaln(c_ctx[None, :], ada_w, ada_b)

    c_glu, c_lx, c_lg = mixer_inputs(ctx, norm1_g, mod_c[0], mod_c[1], w_in, b_in)
    h0 = jnp.zeros((ctx.shape[0], LRU_WIDTH), jnp.float32)
    hf_c = rg_lru_dir(c_lx, lru_p, 0, h0)
    hb_c_rev = rg_lru_dir(c_lx[:, ::-1], lru_p, 1, h0)

    x_glu, x_lx, x_lg = mixer_inputs(x, norm1_g, mod[0], mod[1], w_in, b_in)
    conv_x = conformer_tail(depthwise_conv_grid(x_glu, conv_w) + conv_b, ln_g, ln_b)
    hf = rg_lru_dir(x_lx, lru_p, 0, hf_c[:, -1])
    hb = rg_lru_dir(x_lx[:, ::-1], lru_p, 1, hb_c_rev[:, -1])[:, ::-1]
    x = x + mod[2][:, None, :] * merge_heads(conv_x, hf + hb, x_lg, w_out, b_out)
    x = x + mod[5][:, None, :] * ec_moe(modulate(rms_norm(x, norm2_g), mod[3], mod[4]), *moe_p)

    if update_ctx:
        pad = (CONV_WIDTH - 1) // 2
        conv_c = conformer_tail(depthwise_conv_seq(c_glu, conv_w, (pad, pad)) + conv_b, ln_g, ln_b)
        ctx = ctx + mod_c[2][:, None, :] * merge_heads(conv_c, hf_c + hb_c_rev[:, ::-1], c_lg, w_out, b_out)
        ctx = ctx + mod_c[5][:, None, :] * ec_moe(modulate(rms_norm(ctx, norm2_g), mod_c[3], mod_c[4]), *moe_p)
    return x, ctx


def setup_inputs(seed: int = 0) -> dict:
    key = jax.random.key(seed)
    ks = jax.random.split(key, 28)
    f32 = jnp.float32

    def nrm(k, shape, scale):
        return scale * jax.random.normal(k, shape, f32)

    u = jax.random.uniform(ks[20], (DEPTH, N_DIRS, LRU_WIDTH), f32, 0.9, 0.999)
    a0 = u ** (1.0 / LRU_C)
    return {
        "x": nrm(ks[0], (BATCH, SEQ, D_MODEL), 1.0),
        "c": nrm(ks[1], (BATCH, D_MODEL), 1.0),
        "ctx": nrm(ks[2], (BATCH, CTX_LEN, D_MODEL), 1.0),
        "c_ctx": nrm(ks[3], (D_MODEL,), 1.0),
        "norm1_g": 1.0 + nrm(ks[4], (DEPTH, D_MODEL), 0.02),
        "norm2_g": 1.0 + nrm(ks[5], (DEPTH, D_MODEL), 0.02),
        "ada_w": nrm(ks[6], (DEPTH, D_MODEL, N_MOD * D_MODEL), D_MODEL ** -0.5),
        "ada_b": nrm(ks[7], (DEPTH, N_MOD * D_MODEL), 0.02),
        "w_in": nrm(ks[8], (DEPTH, D_MODEL, D_IN), D_MODEL ** -0.5),
        "b_in": nrm(ks[9], (DEPTH, D_IN), 0.02),
        "conv_dw_w": nrm(ks[10], (DEPTH, CONV_WIDTH, CONV_CH), CONV_WIDTH ** -0.5),
        "conv_dw_b": nrm(ks[11], (DEPTH, CONV_CH), 0.02),
        "conv_ln_g": 1.0 + nrm(ks[12], (DEPTH, CONV_CH), 0.02),
        "conv_ln_b": nrm(ks[13], (DEPTH, CONV_CH), 0.02),
        "lru_conv_w": nrm(ks[14], (DEPTH, N_DIRS, LRU_CONV_WIDTH, LRU_WIDTH), LRU_CONV_WIDTH ** -0.5),
        "lru_conv_b": nrm(ks[15], (DEPTH, N_DIRS, LRU_WIDTH), 0.02),
        "lru_wa": nrm(ks[16], (DEPTH, N_DIRS, LRU_HEADS, LRU_HEAD_DIM, LRU_HEAD_DIM), LRU_HEAD_DIM ** -0.5),
        "lru_ba": nrm(ks[17], (DEPTH, N_DIRS, LRU_WIDTH), 0.02),
        "lru_wi": nrm(ks[18], (DEPTH, N_DIRS, LRU_HEADS, LRU_HEAD_DIM, LRU_HEAD_DIM), LRU_HEAD_DIM ** -0.5),
        "lru_bi": nrm(ks[19], (DEPTH, N_DIRS, LRU_WIDTH), 0.02),
        "lru_lambda": jnp.log(a0) - jnp.log1p(-a0),
        "w_out": nrm(ks[21], (DEPTH, D_MIX, D_MODEL), D_MIX ** -0.5),
        "b_out": nrm(ks[22], (DEPTH, D_MODEL), 0.02),
        "router_w": nrm(ks[23], (DEPTH, D_MODEL, N_EXPERTS), D_MODEL ** -0.5),
        "exp_w_gate": nrm(ks[24], (DEPTH, N_EXPERTS, D_MODEL, D_EXPERT), D_MODEL ** -0.5),
        "exp_w_up": nrm(ks[25], (DEPTH, N_EXPERTS, D_MODEL, D_EXPERT), D_MODEL ** -0.5),
        "exp_w_down": nrm(ks[26], (DEPTH, N_EXPERTS, D_EXPERT, D_MODEL), D_EXPERT ** -0.5),
        "final_norm_g": 1.0 + nrm(ks[27], (D_MODEL,), 0.02),
    }


def reference(x, c, ctx, c_ctx, norm1_g, norm2_g, ada_w, ada_b, w_in, b_in, conv_dw_w, conv_dw_b,
              conv_ln_g, conv_ln_b, lru_conv_w, lru_conv_b, lru_wa, lru_ba, lru_wi, lru_bi, lru_lambda,
              w_out, b_out, router_w, exp_w_gate, exp_w_up, exp_w_down, final_norm_g):
    for l in range(DEPTH):
        conv_p = (conv_dw_w[l], conv_dw_b[l], conv_ln_g[l], conv_ln_b[l])
        lru_p = (lru_conv_w[l], lru_conv_b[l], lru_wa[l], lru_ba[l], lru_wi[l], lru_bi[l], lru_lambda[l])
        moe_p = (router_w[l], exp_w_gate[l], exp_w_up[l], exp_w_down[l])
        x, ctx = hybrid_layer(x, ctx, c, c_ctx, norm1_g[l], norm2_g[l], ada_w[l], ada_b[l], w_in[l], b_in[l],
                              conv_p, lru_p, w_out[l], b_out[l], moe_p, l < DEPTH - 1)
    return rms_norm(x, final_norm_g)
```

```python
import numpy as np
from contextlib import ExitStack
import concourse.bass as bass
import concourse.mybir as mybir
from concourse.bass_utils import run_bass_kernel_spmd

F32 = mybir.dt.float32
BF16 = mybir.dt.bfloat16
I32 = mybir.dt.int32
ALU = mybir.AluOpType
AF = mybir.ActivationFunctionType
AX = mybir.AxisListType

T = 8192
TO = 4096
D = 1024
NE = 16
CAP = 1024
DE = 2816
NF = DE // 128
L = 512
NB = T // L
CTX = 256
TRASH = CAP
NEL = 8
NT = T // 128
CCR = 512
NCC = TO // CCR
NB1 = 10
NBO = 8
NCOL = 16 + 4 + 4 + 4 + 124 + 8 + 8 + 8 + 8 + 32
C_BIN, C_CB, C_LG, C_LB, C_CW = 0, 16, 20, 24, 28
C_LCB, C_BA, C_BI, C_LAM, C_LCW = 152, 160, 168, 176, 184

DEBUG = False


class Buf:
    __slots__ = ("name", "w", "r")

    def __init__(self, name):
        self.name = name
        self.w = None
        self.r = {}


class Sched:
    N_DMA_SEMS = 8

    def __init__(self, nc, es):
        self.nc = nc
        self.engs = {"pe": nc.tensor, "act": nc.scalar, "dve": nc.vector,
                     "pool": nc.gpsimd, "sp": nc.sync}
        self.sems = {}
        self.count = {}
        self.seen = {k: {} for k in self.engs}
        for k in self.engs:
            self.sems[k] = es.enter_context(nc.semaphore("s_" + k))
            self.count[k] = 0
        self.dma_sems = {}
        self.dma_rr = {}
        for q in ("sp", "pool", "act"):
            lst = []
            for i in range(self.N_DMA_SEMS):
                key = "d_%s%d" % (q, i)
                self.sems[key] = es.enter_context(nc.semaphore(key))
                self.count[key] = 0
                lst.append(key)
            self.dma_sems[q] = lst
            self.dma_rr[q] = 0
        self.sems["cc"] = es.enter_context(nc.semaphore("s_cc"))
        self.count["cc"] = 0
        self.n_inst = 0
        self.n_wait = 0

    def coll(self, fn, reads=(), writes=()):
        self._deps("pool", reads, writes)
        ins = fn()
        self.count["cc"] += 1
        ins.then_inc(self.sems["cc"])
        self._mark("cc", self.count["cc"], reads, writes)
        self.n_inst += 1
        return ins

    def _wait(self, eng, key, val):
        if val <= 0:
            return
        s = self.seen[eng]
        if s.get(key, 0) >= val:
            return
        self.engs[eng].wait_ge(self.sems[key], val)
        s[key] = val
        self.n_wait += 1

    def _deps(self, eng, reads, writes):
        for b in reads:
            if b.w is not None:
                self._wait(eng, b.w[0], b.w[1])
        for b in writes:
            if b.w is not None and not (eng == "pe" and b.w[0] == "pe"):
                self._wait(eng, b.w[0], b.w[1])
            for k, v in b.r.items():
                self._wait(eng, k, v)

    def _mark(self, key, val, reads, writes):
        for b in reads:
            if b.r.get(key, 0) < val:
                b.r[key] = val
        for b in writes:
            b.w = (key, val)
            b.r = {}

    def op(self, eng, fn, reads=(), writes=()):
        self._deps(eng, reads, writes)
        ins = fn()
        self.count[eng] += 1
        ins.then_inc(self.sems[eng], 1)
        self._mark(eng, self.count[eng], reads, writes)
        self.n_inst += 1
        return ins

    def dma(self, q, fn, reads=(), writes=()):
        key = self.dma_sems[q][self.dma_rr[q] % self.N_DMA_SEMS]
        self.dma_rr[q] += 1
        self._wait(q, key, self.count[key])
        self._deps(q, reads, writes)
        ins = fn()
        self.count[key] += 16
        ins.then_inc(self.sems[key], 16)
        self._mark(key, self.count[key], reads, writes)
        self.n_inst += 1
        return ins

    def barrier(self):
        for e in self.engs:
            for k in self.sems:
                if k != e:
                    self._wait(e, k, self.count[k])
            self._wait(e, e, self.count[e])


class _Stop(Exception):
    pass


def build_nc(dbg=False, stop_after=99):
    nc = bass.Bass("TRN2", target_bir_lowering=False)

    def din(name, shape, dt=F32):
        return nc.dram_tensor(name, list(shape), dt, kind="ExternalInput").ap()

    def dscr(name, shape, dt):
        return nc.dram_tensor(name, list(shape), dt, kind="Internal").ap()

    x_d = din("xb", [T, D])
    ctx_d = din("ctxb", [CTX, D])
    cc_d = din("cc", [128, 8, 2])
    adaw_d = din("ada_w", [D, 6 * D])
    adab_d = din("ada_b2", [2, 6 * D])
    rowp_d = din("rowp", [128, 4, D])
    colp_d = din("colp", [128, NCOL])
    win_d = din("w_in", [D, 2048])
    wout_d = din("w_out", [D, D])
    rw_d = din("router_w", [128, 8, NE])
    gw_d = din("gw", [128, 2, 2, 4, 128])
    if stop_after >= 6:
        wg_d = din("wg", [NEL, NF, 128, 8, 128])
        wu_d = din("wu", [NEL, NF, 128, 8, 128])
        wd_d = din("wd", [NEL, DE, D])
    pidx_d = din("pidx", [128, 40], I32)
    out_d = nc.dram_tensor("out", [TO, D], F32, kind="ExternalOutput").ap()

    lx_s = dscr("lx_s", [4, 128, T + 6], BF16)
    glg_s = dscr("glg_s", [4, 128, T], BF16)
    hf_s = dscr("hf_s", [4, 128, T], BF16)
    cx_s = dscr("cx_s", [4, 128, T], BF16)
    acc_s = dscr("acc_s", [T + TRASH, D], F32)
    aff_s = dscr("aff_s", [T + TRASH, NE], F32)
    ccsrc = [nc.dram_tensor("ccsrc%d" % k, [128, 4 * D], F32, kind="Internal") for k in range(NCC)]
    ccdst = [nc.dram_tensor("ccdst%d" % k, [256, 4 * D], F32, kind="Internal") for k in range(NCC)]
    xe_s = [dscr("xe_s%d" % e, [CAP, 513], I32) for e in range(NEL)]
    mod_s = dscr("mod_s", [2, 6 * D], F32)
    v_s = [dscr("v_s%d" % k, [CCR, 513], I32) for k in range(NCC)]
    ccvd = [nc.dram_tensor("ccvd%d" % k, [2 * CCR, 513], I32, kind="Internal") for k in range(NCC)]
    ccLs = nc.dram_tensor("ccLs", [128, 32 * NE], F32, kind="Internal")
    ccLd = nc.dram_tensor("ccLd", [256, 32 * NE], F32, kind="Internal")
    vp_s = dscr("vp_s", [TO, 513], I32)
    cc1s = nc.dram_tensor("cc1s", [128, 4], F32, kind="Internal")
    cc1d = nc.dram_tensor("cc1d", [256, 4], F32, kind="Internal")

    dbg_out = {}

    def ddbg(name, shape, dt=F32):
        if dbg:
            dbg_out[name] = nc.dram_tensor("dbg_" + name, list(shape), dt, kind="ExternalOutput").ap()
            return dbg_out[name]
        return None

    es = ExitStack()
    try:
      with es:
        S = Sched(nc, es)

        def ck(k):
            if stop_after == k:
                S.barrier()
                print("kernel build (stop %d): insts" % k, S.n_inst, "waits", S.n_wait)
                raise _Stop()

        uid = [0]

        def sb(st, name, shape, dt):
            uid[0] += 1
            return st.enter_context(nc.sbuf_tensor("s%d_%s" % (uid[0], name), list(shape), dt))

        def psum(st, name, shape, dt):
            uid[0] += 1
            return st.enter_context(nc.psum_tensor("p%d_%s" % (uid[0], name), list(shape), dt))

        V = lambda fn, r=(), w=(): S.op("dve", fn, r, w)
        A = lambda fn, r=(), w=(): S.op("act", fn, r, w)
        P = lambda fn, r=(), w=(): S.op("pe", fn, r, w)
        G = lambda fn, r=(), w=(): S.op("pool", fn, r, w)
        DMA = lambda fn, r=(), w=(), q="sp": S.dma(q, fn, r, w)

        b_lx = [Buf("lx_s%d" % j) for j in range(NB + 1)]
        b_glg = [Buf("glg%d" % j) for j in range(NB)]
        b_hf = [Buf("hf%d" % j) for j in range(NB)]
        b_cx = [Buf("cx%d" % j) for j in range(NB)]
        b_acc = Buf("acc")
        b_affs = Buf("affs")
        b_xe = [Buf("xe%d" % e) for e in range(NEL)]
        b_ccsrc = [Buf("ccsrc%d" % k) for k in range(NCC)]
        b_ccdst = [Buf("ccdst%d" % k) for k in range(NCC)]
        b_mods = Buf("mod_s")
        b_vs = [Buf("v_s%d" % k) for k in range(NCC)]
        b_ccvd = [Buf("ccvd%d" % k) for k in range(NCC)]
        b_vp = [Buf("vp%d" % g) for g in range(32)]
        b_ccLs = Buf("ccLs"); b_ccLd = Buf("ccLd"); b_cc1s = Buf("cc1s"); b_cc1d = Buf("cc1d")

        ident = sb(es, "ident", [128, 128], BF16); b_ident = Buf("ident")
        identf = sb(es, "identf", [128, 128], F32); b_identf = Buf("identf")
        colp = sb(es, "colp", [128, NCOL], F32); b_colp = Buf("colp")
        cst = sb(es, "cst", [128, 4], F32); b_cst = Buf("cst")
        sel = sb(es, "sel", [2, 2, 128], F32); b_sel = Buf("sel")
        ssq = sb(es, "ssq", [128, 64], F32); b_ssq = Buf("ssq")
        rstd = sb(es, "rstd", [128, 64], F32); b_rstd = Buf("rstd")
        mr_t = (sb(es, "mr", [2, D], F32), Buf("mr"))
        pidx_t = sb(es, "pidx_t", [128, 40], I32); b_pidxt = Buf("pidx_t")
        DMA(lambda: nc.sync.dma_start(out=pidx_t[:], in_=pidx_d[:, :]), w=[b_pidxt])
        groups = [[0, 1], [2, 3], [4, 5], [6, 7]]
        reg_c1 = nc.gpsimd.to_reg(255)
        reg_cL = nc.gpsimd.to_reg(2 * TO - 1)
        reg_cc = nc.gpsimd.to_reg(2 * CCR - 1)
        big = es.enter_context(ExitStack())
        logit = sb(big, "logit", [128, 64, NE], F32); b_logit = Buf("logit")
        idx = sb(big, "idx", [128, NT, NE], I32); b_idx = Buf("idx")
        lru_st = es.enter_context(ExitStack())
        clam = sb(lru_st, "clam", [128, 2, 8], F32); b_clam = Buf("clam")
        carry = sb(lru_st, "carry", [128, 8], F32); b_carry = Buf("carry")
        hbias = sb(lru_st, "hbias", [128, 16], F32); b_hbias = Buf("hbias")
        dg4 = sb(lru_st, "dg4", [128, 32, 128], BF16); b_dg4 = Buf("dg4")
        gwb = sb(lru_st, "gwb", [128, 16, 128], BF16); b_gwb = Buf("gwb")
        scb_h = [None, None]
        shb_h = [None, None]
        b_scb = Buf("scb")
        b_shb = Buf("shb")

        G(lambda: nc.gpsimd.memset(identf[:], 0.0), w=[b_identf])
        G(lambda: nc.gpsimd.affine_select(out=identf[:], in_=identf[:], pattern=[[-1, 128]],
                                          compare_op=ALU.not_equal, fill=1.0, base=0,
                                          channel_multiplier=1), r=[b_identf], w=[b_identf])
        V(lambda: nc.vector.tensor_copy(out=ident[:], in_=identf[:]), r=[b_identf], w=[b_ident])
        DMA(lambda: nc.sync.dma_start(out=colp[:], in_=colp_d[:, :]), w=[b_colp])
        V(lambda: nc.vector.memset(cst[:, 0:1], 1e-6), w=[b_cst])
        V(lambda: nc.vector.memset(cst[:, 1:2], 1e-5), w=[b_cst])
        V(lambda: nc.vector.memset(cst[:, 2:3], 1.0), w=[b_cst])
        V(lambda: nc.vector.memset(cst[:, 3:4], 0.25), w=[b_cst])

        def load_rowp(st, r):
            t = sb(st, "rowp%d" % r, [128, D], F32)
            b = Buf("rowp%d" % r)
            DMA(lambda: nc.sync.dma_start(out=t[:], in_=rowp_d[:, r, :]), w=[b])
            return t, b

        with ExitStack() as st:
            zt = sb(st, "zt", [128, 4, 3], BF16); b_zt = Buf("zt")
            V(lambda: nc.vector.memset(zt[:], 0.0), w=[b_zt])
            DMA(lambda: nc.sync.dma_start(out=lx_s.rearrange("c p t -> p c t")[:, :, 0:3], in_=zt[:]),
                r=[b_zt], w=[b_lx[0]])
            DMA(lambda: nc.sync.dma_start(out=lx_s.rearrange("c p t -> p c t")[:, :, T + 3:T + 6], in_=zt[:]),
                r=[b_zt], w=[b_lx[NB]])
            S.barrier()

        with ExitStack() as st:
            modrow = sb(st, "modrow", [2, 6 * D], F32); b_mod = Buf("modrow")
            cc = sb(st, "cc", [128, 8, 2], F32); b_cc = Buf("cc")
            cs = sb(st, "cs", [128, 8, 2], F32); b_cs = Buf("cs")
            adab = sb(st, "adab", [2, 6 * D], F32); b_adab = Buf("adab")
            seli = sb(st, "seli", [2, 2, 128], I32); b_seli = Buf("seli")
            awt = [sb(st, "awt%d" % i, [128, 8, 512], F32) for i in range(2)]
            b_awt = [Buf("awt%d" % i) for i in range(2)]
            pm = [psum(st, "pm%d" % i, [128, 512], F32) for i in range(2)]
            b_pm = [Buf("pm%d" % i) for i in range(2)]
            lam_t = sb(st, "lam_t", [128, 8], F32); b_lam = Buf("lam_t")

            DMA(lambda: nc.sync.dma_start(out=cc[:], in_=cc_d[:, :, :]), w=[b_cc])
            DMA(lambda: nc.sync.dma_start(out=adab[:], in_=adab_d[:, :]), w=[b_adab])
            A(lambda: nc.scalar.activation(out=cs[:], in_=cc[:], func=AF.Silu), r=[b_cc], w=[b_cs])
            G(lambda: nc.gpsimd.iota(seli[:], pattern=[[-1, 2], [0, 128]], base=0, channel_multiplier=1),
              w=[b_seli])
            V(lambda: nc.vector.tensor_single_scalar(out=sel[:], in_=seli[:], scalar=0.0, op=ALU.is_equal),
              r=[b_seli], w=[b_sel])
            A(lambda: nc.scalar.activation(out=lam_t[:], in_=colp[:, C_LAM:C_LAM + 8], func=AF.Exp, scale=-1.0),
              r=[b_colp], w=[b_lam])
            A(lambda: nc.scalar.activation(out=lam_t[:], in_=lam_t[:], func=AF.Ln, bias=cst[:, 2:3], scale=1.0),
              r=[b_lam, b_cst], w=[b_lam])
            V(lambda: nc.vector.tensor_scalar(out=clam[:, 0, :], in0=lam_t[:], scalar1=-4.0, scalar2=None,
                                              op0=ALU.mult), r=[b_lam], w=[b_clam])
            V(lambda: nc.vector.tensor_scalar(out=clam[:, 1, :], in0=lam_t[:], scalar1=-8.0, scalar2=None,
                                              op0=ALU.mult), r=[b_lam], w=[b_clam])
            V(lambda: nc.vector.tensor_scalar(out=hbias[:], in0=colp[:, C_BA:C_BA + 16], scalar1=0.5, scalar2=None,
                                              op0=ALU.mult), r=[b_colp], w=[b_hbias])
            for n in range(12):
                i = n % 2
                DMA(lambda n=n, i=i: nc.sync.dma_start(
                    out=awt[i][:], in_=adaw_d.rearrange("(k p) n -> p k n", p=128)[:, :, n * 512:(n + 1) * 512]),
                    w=[b_awt[i]])
                for k in range(8):
                    P(lambda i=i, k=k: nc.tensor.matmul(pm[i][0:2, :], lhsT=cs[:, k, :], rhs=awt[i][:, k, :],
                                                        start=(k == 0), stop=(k == 7)),
                      r=[b_cs, b_awt[i]], w=[b_pm[i]])
                V(lambda n=n, i=i: nc.vector.tensor_tensor(out=modrow[:, n * 512:(n + 1) * 512], in0=pm[i][0:2, :],
                                                           in1=adab[:, n * 512:(n + 1) * 512], op=ALU.add),
                  r=[b_pm[i], b_adab], w=[b_mod])

            for d in range(2):
                for c in range(4):
                    for k in range(4):
                        col = C_LCW + d * 16 + c * 4 + k
                        V(lambda d=d, c=c, k=k, col=col: nc.vector.tensor_scalar(
                            out=dg4[:, d * 16 + c * 4 + k, :], in0=identf[:], scalar1=colp[:, col:col + 1],
                            scalar2=None, op0=ALU.mult), r=[b_identf, b_colp], w=[b_dg4])
            gwf = sb(st, "gwf", [128, 16, 128], F32); b_gwf = Buf("gwf")
            DMA(lambda: nc.sync.dma_start(out=gwf[:], in_=gw_d.rearrange("p d g c q -> p (d g c) q")), w=[b_gwf])
            V(lambda: nc.vector.tensor_copy(out=gwb[:], in_=gwf[:]), r=[b_gwf], w=[b_gwb])
            DMA(lambda: nc.sync.dma_start(out=mod_s[:, :], in_=modrow[:]), r=[b_mod], w=[b_mods])
            if dbg:
                dd = ddbg("modrow", [2, 6 * D])
                DMA(lambda: nc.sync.dma_start(out=dd[:, :], in_=modrow[:]), r=[b_mod])
            S.barrier()
            ck(0)

        def make_bc(st_ps, dst, b_dst, j, off, mode, other=None, b_other=None):
            mr, b_mr = mr_t
            DMA(lambda: nc.sync.dma_start(out=mr[:], in_=mod_s[:, off:off + D]), r=[b_mods], w=[b_mr])
            for n in range(2):
                pb, b_pb = st_ps[n]
                P(lambda n=n, pb=pb: nc.tensor.matmul(pb[:], lhsT=sel[:, j, :],
                                                      rhs=mr[:, n * 512:(n + 1) * 512],
                                                      start=True, stop=True),
                  r=[b_sel, b_mr], w=[b_pb])
                sl = slice(n * 512, (n + 1) * 512)
                if mode == "copy":
                    A(lambda pb=pb, sl=sl: nc.scalar.copy(out=dst[:, sl], in_=pb[:]), r=[b_pb], w=[b_dst])
                elif mode == "1p_mul":
                    V(lambda pb=pb, sl=sl: nc.vector.scalar_tensor_tensor(
                        out=dst[:, sl], in0=pb[:], scalar=1.0, in1=other[:, sl], op0=ALU.add, op1=ALU.mult),
                      r=[b_pb, b_other], w=[b_dst])
                elif mode == "mul":
                    V(lambda pb=pb, sl=sl: nc.vector.tensor_tensor(out=dst[:, sl], in0=pb[:], in1=other[:, sl],
                                                                   op=ALU.mult), r=[b_pb, b_other], w=[b_dst])

        def norm_tile(src_ap, xt, b_xt, gidx, tmp, b_tmp, ub, b_ub, pT, b_pT, uT, b_uT, col0, q="sp"):
            norm_pre(src_ap, xt, b_xt, gidx, tmp, b_tmp, ub, b_ub)
            norm_post(ub, b_ub, pT, b_pT, uT, b_uT, col0)

        def norm_pre(src_ap, xt, b_xt, gidx, tmp, b_tmp, ub, b_ub, q="sp"):
            DMA(lambda: nc.sync.dma_start(out=xt[:], in_=src_ap), w=[b_xt], q=q)
            A(lambda: nc.scalar.activation(out=tmp[:], in_=xt[:], func=AF.Square, accum_out=ssq[:, gidx:gidx + 1]),
              r=[b_xt], w=[b_tmp, b_ssq])
            A(lambda: nc.scalar.activation(out=rstd[:, gidx:gidx + 1], in_=ssq[:, gidx:gidx + 1], func=AF.Sqrt,
                                           scale=1.0 / D, bias=epsr[:, 0:1]), r=[b_ssq, b_eps], w=[b_rstd])
            V(lambda: nc.vector.reciprocal(out=rstd[:, gidx:gidx + 1], in_=rstd[:, gidx:gidx + 1]),
              r=[b_rstd], w=[b_rstd])
            V(lambda: nc.vector.scalar_tensor_tensor(out=tmp[:], in0=xt[:], scalar=rstd[:, gidx:gidx + 1],
                                                     in1=scb_h[0][:], op0=ALU.mult, op1=ALU.mult),
              r=[b_xt, b_rstd, b_scb], w=[b_tmp])
            V(lambda: nc.vector.tensor_tensor(out=ub[:], in0=tmp[:], in1=shb_h[0][:], op=ALU.add),
              r=[b_tmp, b_shb], w=[b_ub])

        def norm_post(ub, b_ub, pT, b_pT, uT, b_uT, col0):
            for k in range(8):
                P(lambda k=k: nc.tensor.transpose(out=pT[:, k, :], in_=ub[:, k * 128:(k + 1) * 128],
                                                  identity=ident[:]), r=[b_ub, b_ident], w=[b_pT])
            A(lambda: nc.scalar.copy(out=uT[:, :, col0:col0 + 128], in_=pT[:]), r=[b_pT], w=[b_uT])

        epsr, b_eps = cst, b_cst

        class Lru:
            def __init__(self, t, lxw, b_lxw, Lb, d, hout, b_hout, pxs, b_pxs):
                self.t, self.lxw, self.b_lxw, self.Lb, self.d = t, lxw, b_lxw, Lb, d
                self.hout, self.b_hout, self.pxs, self.b_pxs = hout, b_hout, pxs, b_pxs

            def stageA(self, c):
                self.stageA1(c)
                self.stageA2(c)

            def stageA1(self, c):
                t, lxw, b_lxw, Lb, d = self.t, self.lxw, self.b_lxw, self.Lb, self.d
                dc = d * 4 + c
                ns = len(self.pxs) // 3
                o3 = (c % ns) * 3
                px, b_px = self.pxs[o3 + 0], self.b_pxs[o3 + 0]
                pr, b_pr = self.pxs[o3 + 1], self.b_pxs[o3 + 1]
                pi, b_pi = self.pxs[o3 + 2], self.b_pxs[o3 + 2]
                xcf, b_xcf = t["xcf"][c % 2]
                xcb, b_xcb = t["xcb"][c % 2]
                tr, b_tr = t["tr"][c % 2]
                ti, b_ti = t["ti"][c % 2]
                a4, b_a4 = t["a4"]; om4, b_om4 = t["om4"]; ix4, b_ix4 = t["ix4"]
                for k in range(4):
                    o = k if d == 0 else 3 - k
                    P(lambda k=k, o=o: nc.tensor.matmul(px[:, 0:Lb], lhsT=dg4[:, d * 16 + c * 4 + k, :],
                                                        rhs=lxw[:, c, o:o + Lb], start=(k == 0), stop=(k == 3)),
                      r=[b_dg4, b_lxw], w=[b_px])
                A(lambda: nc.scalar.activation(out=xcf[:, 0:Lb], in_=px[:, 0:Lb], func=AF.Identity,
                                               bias=colp[:, C_LCB + dc:C_LCB + dc + 1], scale=1.0),
                  r=[b_px, b_colp], w=[b_xcf])
                V(lambda: nc.vector.tensor_copy(out=xcb[:, 0:Lb], in_=xcf[:, 0:Lb]), r=[b_xcf], w=[b_xcb])

            def stageA2(self, c):
                t, lxw, b_lxw, Lb, d = self.t, self.lxw, self.b_lxw, self.Lb, self.d
                dc = d * 4 + c
                ns = len(self.pxs) // 3
                o3 = (c % ns) * 3
                pr, b_pr = self.pxs[o3 + 1], self.b_pxs[o3 + 1]
                pi, b_pi = self.pxs[o3 + 2], self.b_pxs[o3 + 2]
                xcf, b_xcf = t["xcf"][c % 2]
                xcb, b_xcb = t["xcb"][c % 2]
                tr, b_tr = t["tr"][c % 2]
                ti, b_ti = t["ti"][c % 2]
                a4, b_a4 = t["a4"]; om4, b_om4 = t["om4"]; ix4, b_ix4 = t["ix4"]
                P(lambda: nc.tensor.matmul(pr[:, 0:Lb], lhsT=gwb[:, d * 8 + 0 * 4 + c, :], rhs=xcb[:, 0:Lb],
                                           start=True, stop=True), r=[b_gwb, b_xcb], w=[b_pr])
                P(lambda: nc.tensor.matmul(pi[:, 0:Lb], lhsT=gwb[:, d * 8 + 1 * 4 + c, :], rhs=xcb[:, 0:Lb],
                                           start=True, stop=True), r=[b_gwb, b_xcb], w=[b_pi])
                A(lambda: nc.scalar.activation(out=tr[:, 0:Lb], in_=pr[:, 0:Lb], func=AF.Tanh,
                                               bias=hbias[:, dc:dc + 1], scale=0.5), r=[b_pr, b_hbias], w=[b_tr])
                A(lambda: nc.scalar.activation(out=ti[:, 0:Lb], in_=pi[:, 0:Lb], func=AF.Tanh,
                                               bias=hbias[:, 8 + dc:8 + dc + 1], scale=0.5),
                  r=[b_pi, b_hbias], w=[b_ti])
                A(lambda: nc.scalar.activation(out=a4[:, c, 0:Lb], in_=tr[:, 0:Lb], func=AF.Exp,
                                               scale=clam[:, 0, dc:dc + 1], bias=clam[:, 0, dc:dc + 1]),
                  r=[b_tr, b_clam], w=[b_a4[c]])
                A(lambda: nc.scalar.activation(out=om4[:, c, 0:Lb], in_=tr[:, 0:Lb], func=AF.Exp,
                                               scale=clam[:, 1, dc:dc + 1], bias=clam[:, 1, dc:dc + 1]),
                  r=[b_tr, b_clam], w=[b_om4[c]])
                V(lambda: nc.vector.scalar_tensor_tensor(out=ix4[:, c, 0:Lb], in0=ti[:, 0:Lb], scalar=1.0,
                                                         in1=xcf[:, 0:Lb], op0=ALU.add, op1=ALU.mult),
                  r=[b_ti, b_xcf], w=[b_ix4[c]])

            def stageB(self):
                t, Lb = self.t, self.Lb
                om4, b_om4 = t["om4"]
                A(lambda: nc.scalar.activation(out=om4[:, :, 0:Lb], in_=om4[:, :, 0:Lb], func=AF.Sqrt,
                                               scale=-0.25, bias=cst[:, 3:4]), r=list(b_om4) + [b_cst], w=list(b_om4))

            def stageC(self, c):
                t, Lb, d, hout, b_hout = self.t, self.Lb, self.d, self.hout, self.b_hout
                dc = d * 4 + c
                a4, b_a4 = t["a4"]; om4, b_om4 = t["om4"]; ix4, b_ix4 = t["ix4"]
                V(lambda: nc.vector.tensor_tensor(out=ix4[:, c, 0:Lb], in0=ix4[:, c, 0:Lb], in1=om4[:, c, 0:Lb],
                                                  op=ALU.mult), r=[b_ix4[c], b_om4[c]], w=[b_ix4[c]])
                if d == 0:
                    V(lambda: nc.vector.tensor_tensor_scan(out=hout[:, c, 0:Lb], data0=a4[:, c, 0:Lb],
                                                           data1=ix4[:, c, 0:Lb], initial=carry[:, dc:dc + 1],
                                                           op0=ALU.mult, op1=ALU.add),
                      r=[b_a4[c], b_ix4[c], b_carry], w=[b_hout])
                    V(lambda: nc.vector.tensor_copy(out=carry[:, dc:dc + 1], in_=hout[:, c, Lb - 1:Lb]),
                      r=[b_hout], w=[b_carry])
                else:
                    V(lambda: nc.vector.tensor_tensor_scan(out=hout[:, c, 0:Lb][:, ::-1],
                                                           data0=a4[:, c, 0:Lb][:, ::-1],
                                                           data1=ix4[:, c, 0:Lb][:, ::-1],
                                                           initial=carry[:, dc:dc + 1], op0=ALU.mult, op1=ALU.add),
                      r=[b_a4[c], b_ix4[c], b_carry], w=[b_hout])
                    V(lambda: nc.vector.tensor_copy(out=carry[:, dc:dc + 1], in_=hout[:, c, 0:1]),
                      r=[b_hout], w=[b_carry])

            def run(self):
                for c in range(4):
                    self.stageA(c)
                self.stageB()
                for c in range(4):
                    self.stageC(c)

        def lru_block(st_t, lxw, b_lxw, Lb, d, hout, b_hout, pxs, b_pxs):
            Lru(st_t, lxw, b_lxw, Lb, d, hout, b_hout, pxs, b_pxs).run()

        def lru_tiles(st, W=L):
            t = {}
            for nm, dt in (("xcf", F32), ("xcb", BF16), ("tr", F32), ("ti", F32)):
                t[nm] = [(sb(st, "lr_%s%d" % (nm, i), [128, W], dt), Buf("lr_%s%d" % (nm, i))) for i in range(2)]
            for nm in ("a4", "om4", "ix4"):
                t[nm] = (sb(st, "lr_" + nm, [128, 4, W], F32), [Buf("lr_%s%d" % (nm, c)) for c in range(4)])
            return t

        glu_st = es.enter_context(ExitStack())
        glu = sb(glu_st, "glu", [128, 4, T], BF16); b_glu = [Buf("glu%d" % j) for j in range(NB)]
        with ExitStack() as st:
            winb = sb(st, "winb", [128, 8, 2048], BF16); b_winb = Buf("winb")
            scb = sb(st, "scb", [128, D], F32); shb = sb(st, "shb", [128, D], F32)
            scb_h[0] = scb; shb_h[0] = shb
            rp0, b_rp0 = load_rowp(st, 0)
            st2 = ExitStack()
            wst = [sb(st2, "wst%d" % i, [128, 8, 256], F32) for i in range(2)]
            b_wst = [Buf("wst%d" % i) for i in range(2)]
            for n in range(8):
                i = n % 2
                DMA(lambda n=n, i=i: nc.sync.dma_start(
                    out=wst[i][:], in_=win_d.rearrange("(k p) n -> p k n", p=128)[:, :, n * 256:(n + 1) * 256]),
                    w=[b_wst[i]])
                if n % 2 == 0:
                    V(lambda n=n, i=i: nc.vector.tensor_copy(out=winb[:, :, n * 256:(n + 1) * 256], in_=wst[i][:]),
                      r=[b_wst[i]], w=[b_winb])
                else:
                    A(lambda n=n, i=i: nc.scalar.copy(out=winb[:, :, n * 256:(n + 1) * 256], in_=wst[i][:]),
                      r=[b_wst[i]], w=[b_winb])
            S.barrier()
            st2.close()
            xts = [sb(st, "xt%d" % i, [128, D], F32) for i in range(2)]
            b_xts = [Buf("xt%d" % i) for i in range(2)]
            tmp = sb(st, "tmp", [128, D], F32); b_tmp = Buf("tmp")
            ubs = [sb(st, "ub%d" % i, [128, D], BF16) for i in range(4)]
            b_ubs = [Buf("ub%d" % i) for i in range(4)]
            uTs = [sb(st, "uT%d" % i, [128, 8, L], BF16) for i in range(2)]
            b_uTs = [Buf("uT%d" % i) for i in range(2)]
            pT = psum(st, "pT", [128, 8, 128], BF16); b_pT = Buf("pT")
            pbs = [(psum(st, "pb%d" % i, [128, 512], F32), Buf("pb%d" % i)) for i in range(4)]
            pxs = [psum(st, "px%d" % i, [128, 512], F32) for i in range(3)]
            b_pxs = [Buf("px%d" % i) for i in range(3)]
            stc = ExitStack()
            lt = lru_tiles(stc, CTX)

            make_bc(pbs[0:2], scb, b_scb, 1, 1 * D, "1p_mul", rp0, b_rp0)
            make_bc(pbs[2:4], shb, b_shb, 1, 0 * D, "copy")
            clx = sb(stc, "clx", [128, 4, CTX + 6], BF16); b_clx = Buf("clx")
            chh = sb(stc, "chh", [128, 4, CTX], F32); b_chh = Buf("chh")
            V(lambda: nc.vector.memset(clx[:], 0.0), w=[b_clx])
            V(lambda: nc.vector.memset(carry[:], 0.0), w=[b_carry])
            for tt in range(2):
                norm_tile(ctx_d[tt * 128:(tt + 1) * 128, :], xts[tt], b_xts[tt], tt, tmp, b_tmp, ubs[tt], b_ubs[tt],
                          pT, b_pT, uTs[0], b_uTs[0], tt * 128)
            for c in range(4):
                m = 8 + c
                pb, b_pb = pbs[c % 4]
                for k in range(8):
                    P(lambda k=k, m=m, pb=pb: nc.tensor.matmul(pb[:, 0:CTX], lhsT=winb[:, k, m * 128:(m + 1) * 128],
                                                               rhs=uTs[0][:, k, 0:CTX], start=(k == 0), stop=(k == 7)),
                      r=[b_winb, b_uTs[0]], w=[b_pb])
                A(lambda c=c, m=m, pb=pb: nc.scalar.activation(out=clx[:, c, 3:3 + CTX], in_=pb[:, 0:CTX],
                                                               func=AF.Identity, bias=colp[:, m:m + 1], scale=1.0),
                  r=[b_pb, b_colp], w=[b_clx])
            lru_block(lt, clx[:, :, 0:CTX + 3], b_clx, CTX, 0, chh, b_chh, pxs, b_pxs)
            lru_block(lt, clx[:, :, 3:CTX + 6], b_clx, CTX, 1, chh, b_chh, pxs, b_pxs)
            if dbg:
                dd = ddbg("carry0", [128, 8])
                DMA(lambda: nc.sync.dma_start(out=dd[:, :], in_=carry[:]), r=[b_carry])
            S.barrier()
            stc.close()
            ck(0.5)
            sig = [sb(st, "sig%d" % i, [128, L], F32) for i in range(2)]
            b_sig = [Buf("sig%d" % i) for i in range(2)]
            lxst = [sb(st, "lxst%d" % i, [128, 4, L], BF16) for i in range(2)]
            b_lxst = [Buf("lxst%d" % i) for i in range(2)]
            ggst = [sb(st, "ggst%d" % i, [128, 4, L], BF16) for i in range(2)]
            b_ggst = [Buf("ggst%d" % i) for i in range(2)]

            make_bc(pbs[0:2], scb, b_scb, 0, 1 * D, "1p_mul", rp0, b_rp0)
            make_bc(pbs[2:4], shb, b_shb, 0, 0 * D, "copy")

            def npre(jb, tt):
                g = jb * 4 + tt
                norm_pre(x_d[g * 128:(g + 1) * 128, :], xts[g % 2], b_xts[g % 2], g, tmp, b_tmp, ubs[tt], b_ubs[tt])

            def npost(jb, tt):
                norm_post(ubs[tt], b_ubs[tt], pT, b_pT, uTs[jb % 2], b_uTs[jb % 2], tt * 128)

            for tt in range(4):
                npre(0, tt)
                npost(0, tt)
            for j in range(NB1):
                uT, b_uT = uTs[j % 2], b_uTs[j % 2]
                order = [4, 0, 5, 1, 6, 2, 7, 3, 8, 9, 10, 11, 12, 13, 14, 15]
                for qi, m in enumerate(order):
                    if qi % 4 == 0 and j + 1 < NB1:
                        npre(j + 1, qi // 4)
                        if qi >= 4:
                            npost(j + 1, qi // 4 - 1)
                    pb, b_pb = pbs[qi % 4]
                    for k in range(8):
                        P(lambda k=k, m=m, pb=pb: nc.tensor.matmul(pb[:], lhsT=winb[:, k, m * 128:(m + 1) * 128],
                                                                   rhs=uT[:, k, :], start=(k == 0), stop=(k == 7)),
                          r=[b_winb, b_uT], w=[b_pb])
                    if 4 <= m < 8:
                        sg, b_sg = sig[m % 2], b_sig[m % 2]
                        A(lambda m=m, pb=pb, sg=sg: nc.scalar.activation(out=sg[:], in_=pb[:], func=AF.Sigmoid,
                                                                         bias=colp[:, m:m + 1], scale=1.0),
                          r=[b_pb, b_colp], w=[b_sg])
                    elif m < 4:
                        sg, b_sg = sig[m % 2], b_sig[m % 2]
                        V(lambda m=m, pb=pb, sg=sg: nc.vector.scalar_tensor_tensor(
                            out=glu[:, m, j * L:(j + 1) * L], in0=pb[:], scalar=colp[:, m:m + 1], in1=sg[:],
                            op0=ALU.add, op1=ALU.mult), r=[b_pb, b_colp, b_sg], w=[b_glu[j]])
                    elif m < 12:
                        A(lambda m=m, pb=pb: nc.scalar.activation(out=lxst[j % 2][:, m - 8, :], in_=pb[:],
                                                                  func=AF.Identity, bias=colp[:, m:m + 1], scale=1.0),
                          r=[b_pb, b_colp], w=[b_lxst[j % 2]])
                    else:
                        A(lambda m=m, pb=pb: nc.scalar.activation(out=ggst[j % 2][:, m - 12, :], in_=pb[:],
                                                                  func=AF.Gelu_apprx_tanh, bias=colp[:, m:m + 1],
                                                                  scale=1.0),
                          r=[b_pb, b_colp], w=[b_ggst[j % 2]])
                if j + 1 < NB1:
                    npost(j + 1, 3)
                DMA(lambda j=j: nc.sync.dma_start(
                    out=lx_s.rearrange("c p t -> p c t")[:, :, 3 + j * L:3 + (j + 1) * L], in_=lxst[j % 2][:]),
                    r=[b_lxst[j % 2]], w=[b_lx[j]])
                DMA(lambda j=j: nc.sync.dma_start(
                    out=glg_s.rearrange("c p t -> p c t")[:, :, j * L:(j + 1) * L], in_=ggst[j % 2][:]),
                    r=[b_ggst[j % 2]], w=[b_glg[j]])
            if dbg:
                dd = ddbg("glu", [128, 4, T], BF16)
                DMA(lambda: nc.sync.dma_start(out=dd[:, :, :], in_=glu[:]), r=b_glu)
            S.barrier()
            ck(1)

        with ExitStack() as st:
            zt4 = sb(st, "zt4", [128, 2, D], F32); b_zt4 = Buf("zt4")
            V(lambda: nc.vector.memset(zt4[:], 0.0), w=[b_zt4])
            for k in range(2 * NCC):
                DMA(lambda k=k: nc.gpsimd.dma_start(
                    out=acc_s[TO + k * 256:TO + (k + 1) * 256, :].rearrange("(j p) n -> p j n", p=128), in_=zt4[:]),
                    r=[b_zt4], w=[b_acc], q="pool")
            tid = sb(st, "tid", [128, 8, 1], I32); b_tid = Buf("tid")
            G(lambda: nc.gpsimd.iota(tid[:], pattern=[[128, 8], [0, 1]], base=T, channel_multiplier=1), w=[b_tid])
            for e in range(NEL):
                DMA(lambda e=e: nc.gpsimd.dma_start(out=xe_s[e].rearrange("(b p) w -> p b w", p=128)[:, :, 512:513],
                                                    in_=tid[:], allow_slow_non_contiguous=True),
                    r=[b_tid], w=[b_xe[e]], q="pool")
            lt = lru_tiles(st)
            pxs = [psum(st, "px%d" % i, [128, 512], F32) for i in range(6)]
            b_pxs = [Buf("px%d" % i) for i in range(6)]
            lxw = [sb(st, "lxw%d" % i, [128, 4, L + 3], BF16) for i in range(2)]
            b_lxw = [Buf("lxw%d" % i) for i in range(2)]
            hh = [sb(st, "hh%d" % i, [128, 4, L], F32) for i in range(2)]
            b_hh = [Buf("hh%d" % i) for i in range(2)]
            hb16 = [sb(st, "hb16%d" % i, [128, 4, L], BF16) for i in range(2)]
            b_hb16 = [Buf("hb16%d" % i) for i in range(2)]
            def mk_lru(j):
                i = j % 2
                deps = [b_lx[j]] + ([b_lx[j - 1]] if j > 0 else [b_lx[0]])
                DMA(lambda: nc.sync.dma_start(
                    out=lxw[i][:], in_=lx_s.rearrange("c p t -> p c t")[:, :, j * L:j * L + L + 3]),
                    r=deps, w=[b_lxw[i]])
                return Lru(lt, lxw[i], b_lxw[i], L, 0, hh[i], b_hh[i], pxs, b_pxs)

            cur = mk_lru(0)
            for c in range(4):
                cur.stageA(c)
            cur.stageB()
            for j in range(NBO):
                i = j % 2
                nxt = mk_lru(j + 1) if j + 1 < NBO else None
                for c in range(4):
                    cur.stageC(c)
                    if nxt is not None:
                        nxt.stageA(c)
                if nxt is not None:
                    nxt.stageB()
                G(lambda i=i: nc.gpsimd.tensor_copy(out=hb16[i][:], in_=hh[i][:]), r=[b_hh[i]], w=[b_hb16[i]])
                DMA(lambda j=j, i=i: nc.sync.dma_start(
                    out=hf_s.rearrange("c p t -> p c t")[:, :, j * L:(j + 1) * L], in_=hb16[i][:]),
                    r=[b_hb16[i]], w=[b_hf[j]])
                cur = nxt
            DMA(lambda: nc.sync.dma_start(out=cc1s.ap()[:, :], in_=carry[:, 0:4]), r=[b_carry], w=[b_cc1s])
            S.coll(lambda: nc.gpsimd.collective_compute("AllGather", ALU.bypass, replica_groups=groups,
                                                        ins=[cc1s.ap().opt()], outs=[cc1d.ap().opt()]),
                   reads=[b_cc1s], writes=[b_cc1d])
            DMA(lambda: nc.gpsimd.indirect_dma_start(
                out=carry[:, 4:8], out_offset=None, in_=cc1d.ap()[:, :],
                in_offset=bass.IndirectOffsetOnAxis(ap=pidx_t[:, 36:37], axis=0),
                bounds_check=reg_c1, oob_is_err=False), r=[b_pidxt, b_cc1d], w=[b_carry], q="pool")
            S.barrier()
            ck(2)

        with ExitStack() as st:
            dgc = sb(st, "dgc", [128, 124, 128], BF16); b_dgc = Buf("dgc")
            for c in range(4):
                for k in range(31):
                    col = C_CW + c * 31 + k
                    eng = V
                    if eng is V:
                        V(lambda c=c, k=k, col=col: nc.vector.tensor_scalar(
                            out=dgc[:, c * 31 + k, :], in0=identf[:], scalar1=colp[:, col:col + 1], scalar2=None,
                            op0=ALU.mult), r=[b_identf, b_colp], w=[b_dgc])
                    else:
                        G(lambda c=c, k=k, col=col: nc.gpsimd.tensor_scalar(
                            out=dgc[:, c * 31 + k, :], in0=identf[:], scalar1=colp[:, col:col + 1], scalar2=None,
                            op0=ALU.mult), r=[b_identf, b_colp], w=[b_dgc])
            onesf = sb(st, "onesf", [128, 128], F32); b_ones = Buf("onesf")
            V(lambda: nc.vector.memset(onesf[:], 1.0 / 512.0), w=[b_ones])
            pcs = [psum(st, "pc%d" % i, [128, 512], F32) for i in range(4)]
            b_pcs = [Buf("pc%d" % i) for i in range(4)]
            pmean = psum(st, "pmean", [128, 512], F32); b_pmean = Buf("pmean")
            pex2 = psum(st, "pex2", [128, 512], F32); b_pex2 = Buf("pex2")
            yf = [sb(st, "yf%d" % i, [128, 4, L], F32) for i in range(2)]
            b_yf = [Buf("yf%d" % i) for i in range(2)]
            y2 = sb(st, "y2", [128, 4, L], F32); b_y2 = Buf("y2")
            mean = sb(st, "mean", [128, L], F32); b_mean = Buf("mean")
            var = sb(st, "var", [128, L], F32); b_var = Buf("var")
            cxst = [sb(st, "cxst%d" % i, [128, 4, L], BF16) for i in range(2)]
            b_cxst = [Buf("cxst%d" % i) for i in range(2)]
            for j in range(NBO):
                i = j % 2
                r0 = j * 8
                for c in range(4):
                    mms = []
                    for k in [15] + [kk for kk in range(31) if kk != 15]:
                        s_ = k - 15
                        if c < 2:
                            g3 = glu[:, c, :].rearrange("p (r w) -> p r w", w=64)
                            p3 = pcs[c][:].rearrange("p (r w) -> p r w", w=64)
                            rhs = g3[:, r0:r0 + 8, max(0, s_):64 + min(0, s_)]
                            out = p3[:, :, max(0, -s_):64 - max(0, s_)]
                            jl, jh = j, j
                        else:
                            r_lo = max(r0, -s_)
                            r_hi = min(r0 + 8, 128 - s_)
                            if r_lo >= r_hi:
                                continue
                            rhs = glu[:, c, (r_lo + s_) * 64:(r_hi + s_) * 64]
                            out = pcs[c][:, (r_lo - r0) * 64:(r_hi - r0) * 64]
                            jl, jh = (r_lo + s_) // 8, (r_hi + s_ - 1) // 8
                        mms.append((k, rhs, out, jl, jh))
                    for q, (k, rhs, out, jl, jh) in enumerate(mms):
                        P(lambda c=c, k=k, rhs=rhs, out=out, q=q, n=len(mms): nc.tensor.matmul(
                            out, lhsT=dgc[:, c * 31 + k, :], rhs=rhs, start=(q == 0), stop=(q == n - 1)),
                          r=[b_dgc] + b_glu[jl:jh + 1], w=[b_pcs[c]])
                    A(lambda c=c, i=i: nc.scalar.activation(out=yf[i][:, c, :], in_=pcs[c][:], func=AF.Identity,
                                                            bias=colp[:, C_CB + c:C_CB + c + 1], scale=1.0),
                      r=[b_pcs[c], b_colp], w=[b_yf[i]])
                    G(lambda c=c, i=i: nc.gpsimd.tensor_tensor(out=y2[:, c, :], in0=yf[i][:, c, :], in1=yf[i][:, c, :],
                                                               op=ALU.mult), r=[b_yf[i]], w=[b_y2])
                for c in range(4):
                    P(lambda c=c, i=i: nc.tensor.matmul(pmean[:], lhsT=onesf[:], rhs=yf[i][:, c, :],
                                                        start=(c == 0), stop=(c == 3)),
                      r=[b_ones, b_yf[i]], w=[b_pmean])
                for c in range(4):
                    P(lambda c=c: nc.tensor.matmul(pex2[:], lhsT=onesf[:], rhs=y2[:, c, :],
                                                   start=(c == 0), stop=(c == 3)),
                      r=[b_ones, b_y2], w=[b_pex2])
                A(lambda: nc.scalar.copy(out=mean[:], in_=pmean[:]), r=[b_pmean], w=[b_mean])
                A(lambda: nc.scalar.activation(out=var[:], in_=pmean[:], func=AF.Square), r=[b_pmean], w=[b_var])
                V(lambda: nc.vector.tensor_tensor(out=var[:], in0=pex2[:], in1=var[:], op=ALU.subtract),
                  r=[b_pex2, b_var], w=[b_var])
                A(lambda: nc.scalar.activation(out=var[:], in_=var[:], func=AF.Sqrt, bias=epsr[:, 1:2], scale=1.0),
                  r=[b_var, b_eps], w=[b_var])
                V(lambda: nc.vector.reciprocal(out=var[:], in_=var[:]), r=[b_var], w=[b_var])
                for c in range(4):
                    V(lambda c=c, i=i: nc.vector.tensor_tensor(out=yf[i][:, c, :], in0=yf[i][:, c, :], in1=mean[:],
                                                               op=ALU.subtract), r=[b_yf[i], b_mean], w=[b_yf[i]])
                    V(lambda c=c, i=i: nc.vector.tensor_tensor(out=yf[i][:, c, :], in0=yf[i][:, c, :], in1=var[:],
                                                               op=ALU.mult), r=[b_yf[i], b_var], w=[b_yf[i]])
                    A(lambda c=c, i=i: nc.scalar.activation(out=cxst[i][:, c, :], in_=yf[i][:, c, :], func=AF.Silu,
                                                            bias=colp[:, C_LB + c:C_LB + c + 1],
                                                            scale=colp[:, C_LG + c:C_LG + c + 1]),
                      r=[b_yf[i], b_colp], w=[b_cxst[i]])
                DMA(lambda j=j, i=i: nc.sync.dma_start(
                    out=cx_s.rearrange("c p t -> p c t")[:, :, j * L:(j + 1) * L], in_=cxst[i][:]),
                    r=[b_cxst[i]], w=[b_cx[j]])
            S.barrier()
            ck(3)
        glu_st.close()

        with ExitStack() as st:
            woutb = sb(st, "woutb", [128, 8, D], BF16); b_woutb = Buf("woutb")
            scb = sb(st, "scb", [128, D], F32); shb = sb(st, "shb", [128, D], F32)
            scb_h[0] = scb; shb_h[0] = shb
            rp1, b_rp1 = load_rowp(st, 1)
            rp2, b_rp2 = load_rowp(st, 2)
            st2 = ExitStack()
            wst = [sb(st2, "wst%d" % i, [128, 8, 256], F32) for i in range(2)]
            b_wst = [Buf("wst%d" % i) for i in range(2)]
            for n in range(4):
                i = n % 2
                DMA(lambda n=n, i=i: nc.sync.dma_start(
                    out=wst[i][:], in_=wout_d.rearrange("(k p) n -> p k n", p=128)[:, :, n * 256:(n + 1) * 256]),
                    w=[b_wst[i]])
                V(lambda n=n, i=i: nc.vector.tensor_copy(out=woutb[:, :, n * 256:(n + 1) * 256], in_=wst[i][:]),
                  r=[b_wst[i]], w=[b_woutb])
            S.barrier()
            st2.close()
            rwt = sb(st, "rwt", [128, 8, NE], F32); b_rwt = Buf("rwt")
            DMA(lambda: nc.sync.dma_start(out=rwt[:], in_=rw_d[:, :, :]), w=[b_rwt])
            g1t = sb(st, "g1t", [128, D], F32); b_g1t = Buf("g1t")
            bob = sb(st, "bob", [128, D], F32); b_bob = Buf("bob")
            pbs = [(psum(st, "pb%d" % i, [128, 512], F32), Buf("pb%d" % i)) for i in range(2)]
            pxs = [psum(st, "px%d" % i, [128, 512], F32) for i in range(3)]
            b_pxs = [Buf("px%d" % i) for i in range(3)]
            pvT = psum(st, "pvT", [128, 8, 128], F32); b_pvT = Buf("pvT")
            plg = psum(st, "plg", [128, NE], F32); b_plg = Buf("plg")
            make_bc(pbs, g1t, b_g1t, 0, 2 * D, "copy")
            make_bc(pbs, bob, b_bob, 0, 2 * D, "mul", rp2, b_rp2)
            make_bc(pbs, scb, b_scb, 0, 4 * D, "1p_mul", rp1, b_rp1)
            make_bc(pbs, shb, b_shb, 0, 3 * D, "copy")
            vrow = [sb(st, "vrow%d" % i, [128, 513], I32) for i in range(2)]
            b_vrow = [Buf("vrow%d" % i) for i in range(2)]
            lt = lru_tiles(st)
            lxw = [sb(st, "lxw%d" % i, [128, 4, L + 3], BF16) for i in range(2)]
            b_lxw = [Buf("lxw%d" % i) for i in range(2)]
            hh = [sb(st, "hh%d" % i, [128, 4, L], F32) for i in range(2)]
            b_hh = [Buf("hh%d" % i) for i in range(2)]
            hfl = sb(st, "hfl", [128, 4, L], BF16); b_hfl = Buf("hfl")
            ggl = sb(st, "ggl", [128, 4, L], BF16); b_ggl = Buf("ggl")
            ycat = [sb(st, "ycat%d" % i, [128, 8, L], BF16) for i in range(2)]
            b_ycat = [Buf("ycat%d" % i) for i in range(2)]
            xts = [sb(st, "xt%d" % i, [128, D], F32) for i in range(2)]
            b_xts = [Buf("xt%d" % i) for i in range(2)]
            x1 = [sb(st, "x1%d" % i, [128, D], F32) for i in range(4)]
            b_x1 = [Buf("x1%d" % i) for i in range(4)]
            vf = [sb(st, "vf%d" % i, [128, D], F32) for i in range(4)]
            b_vf = [Buf("vf%d" % i) for i in range(4)]
            junkb = sb(st, "junkb", [128, D], BF16); b_junkb = Buf("junkb")
            vT = [sb(st, "vT%d" % i, [128, 8, 128], F32) for i in range(1)] * 2
            b_vT = [Buf("vT%d" % i) for i in range(1)] * 2
            dbg_x1 = ddbg("x1", [T, D]) if dbg else None

            def load_block(j, i):
                DMA(lambda: nc.sync.dma_start(
                    out=lxw[i][:], in_=lx_s.rearrange("c p t -> p c t")[:, :, 3 + j * L:3 + j * L + L + 3]),
                    r=[b_lx[j], b_lx[j + 1]], w=[b_lxw[i]])
                DMA(lambda: nc.sync.dma_start(
                    out=hfl[:], in_=hf_s.rearrange("c p t -> p c t")[:, :, j * L:(j + 1) * L]),
                    r=[b_hf[j]], w=[b_hfl])
                DMA(lambda: nc.sync.dma_start(
                    out=ggl[:], in_=glg_s.rearrange("c p t -> p c t")[:, :, j * L:(j + 1) * L]),
                    r=[b_glg[j]], w=[b_ggl])
                DMA(lambda: nc.sync.dma_start(
                    out=ycat[i][:, 0:4, :], in_=cx_s.rearrange("c p t -> p c t")[:, :, j * L:(j + 1) * L]),
                    r=[b_cx[j]], w=[b_ycat[i]])

            def merge(i, c):
                V(lambda: nc.vector.tensor_tensor(out=hh[i][:, c, :], in0=hh[i][:, c, :], in1=hfl[:, c, :],
                                                  op=ALU.add), r=[b_hh[i], b_hfl], w=[b_hh[i]])
                G(lambda: nc.gpsimd.tensor_tensor(out=ycat[i][:, 4 + c, :], in0=hh[i][:, c, :],
                                                  in1=ggl[:, c, :], op=ALU.mult),
                  r=[b_hh[i], b_ggl], w=[b_ycat[i]])

            def tile_s1(j, i, tt):
                g = j * 4 + tt
                xt, b_xt = xts[g % 2], b_xts[g % 2]
                xx, b_xx = x1[tt], b_x1[tt]
                DMA(lambda: nc.sync.dma_start(out=xt[:], in_=x_d[g * 128:(g + 1) * 128, :]), w=[b_xt])
                G(lambda: nc.gpsimd.tensor_tensor(out=xt[:], in0=xt[:], in1=bob[:], op=ALU.add),
                  r=[b_xt, b_bob], w=[b_xt])
                for n in range(2):
                    pb, b_pb = pbs[n]
                    for k in range(8):
                        P(lambda k=k, n=n, pb=pb: nc.tensor.matmul(
                            pb[:], lhsT=ycat[i][:, k, tt * 128:(tt + 1) * 128],
                            rhs=woutb[:, k, n * 512:(n + 1) * 512], start=(k == 0), stop=(k == 7)),
                          r=[b_ycat[i], b_woutb], w=[b_pb])
                    sl = slice(n * 512, (n + 1) * 512)
                    V(lambda pb=pb, sl=sl: nc.vector.tensor_tensor(out=xx[:, sl], in0=pb[:], in1=g1t[:, sl],
                                                                   op=ALU.mult), r=[b_pb, b_g1t], w=[b_xx])
                V(lambda: nc.vector.tensor_tensor(out=xx[:], in0=xx[:], in1=xt[:], op=ALU.add),
                  r=[b_xx, b_xt], w=[b_xx])
                if True:
                    DMA(lambda: nc.sync.dma_start(out=acc_s[g * 128:(g + 1) * 128, :], in_=xx[:]),
                        r=[b_xx], w=[b_acc])
                if dbg:
                    DMA(lambda: nc.sync.dma_start(out=dbg_x1[g * 128:(g + 1) * 128, :], in_=xx[:]), r=[b_xx])
                A(lambda: nc.scalar.activation(out=junkb[:], in_=xx[:], func=AF.Square,
                                               accum_out=ssq[:, g:g + 1]), r=[b_xx], w=[b_junkb, b_ssq])

            def rstd_block(j):
                g0 = j * 4
                A(lambda: nc.scalar.activation(out=rstd[:, g0:g0 + 4], in_=ssq[:, g0:g0 + 4], func=AF.Sqrt,
                                               scale=1.0 / D, bias=epsr[:, 0:1]), r=[b_ssq, b_eps], w=[b_rstd])
                V(lambda: nc.vector.reciprocal(out=rstd[:, g0:g0 + 4], in_=rstd[:, g0:g0 + 4]),
                  r=[b_rstd], w=[b_rstd])

            def tile_s2(j, tt):
                tile_s2a(j, tt)
                tile_s2b(j, tt)

            def tile_s2a(j, tt):
                g = j * 4 + tt
                xx, b_xx = x1[tt], b_x1[tt]
                v, b_v = vf[tt], b_vf[tt]
                V(lambda: nc.vector.scalar_tensor_tensor(out=v[:], in0=xx[:], scalar=rstd[:, g:g + 1],
                                                         in1=scb[:], op0=ALU.mult, op1=ALU.mult),
                  r=[b_xx, b_rstd, b_scb], w=[b_v])
                V(lambda: nc.vector.tensor_tensor(out=v[:], in0=v[:], in1=shb[:], op=ALU.add),
                  r=[b_v, b_shb], w=[b_v])
                if True:
                    vr, b_vr = vrow[g % 2], b_vrow[g % 2]
                    G(lambda: nc.gpsimd.iota(vr[:, 512:513], pattern=[[0, 1]], base=g * 128,
                                             channel_multiplier=1), w=[b_vr])
                    A(lambda: nc.scalar.copy(out=vr[:, 0:512].bitcast(BF16), in_=v[:]), r=[b_v], w=[b_vr])
                    DMA(lambda: nc.sync.dma_start(out=v_s[g // 4][(g % 4) * 128:(g % 4 + 1) * 128, :], in_=vr[:]),
                        r=[b_vr], w=[b_vs[g // 4]])

            def tile_s2b(j, tt):
                g = j * 4 + tt
                v, b_v = vf[tt], b_vf[tt]
                vt_, b_vt_ = vT[g % 2], b_vT[g % 2]
                for k in range(8):
                    P(lambda k=k: nc.tensor.transpose(out=pvT[:, k, :], in_=v[:, k * 128:(k + 1) * 128],
                                                      identity=identf[:]), r=[b_v, b_identf], w=[b_pvT])
                A(lambda: nc.scalar.copy(out=vt_[:], in_=pvT[:]), r=[b_pvT], w=[b_vt_])
                for k in range(8):
                    P(lambda k=k: nc.tensor.matmul(plg[:], lhsT=vt_[:, k, :], rhs=rwt[:, k, :],
                                                   start=(k == 0), stop=(k == 7)), r=[b_vt_, b_rwt], w=[b_plg])
                A(lambda: nc.scalar.copy(out=logit[:, g, :], in_=plg[:]), r=[b_plg], w=[b_logit])

            load_block(NBO - 1, 0)
            lr0 = Lru(lt, lxw[0], b_lxw[0], L, 1, hh[0], b_hh[0], pxs, b_pxs)
            lr0.run()
            for c in range(4):
                merge(0, c)
            for jj in range(NBO):
                j = NBO - 1 - jj
                i = jj % 2
                nxt = None
                if jj + 1 < NBO:
                    i2 = (jj + 1) % 2
                    load_block(j - 1, i2)
                    nxt = Lru(lt, lxw[i2], b_lxw[i2], L, 1, hh[i2], b_hh[i2], pxs, b_pxs)
                for tt in range(4):
                    if nxt is not None:
                        nxt.stageA1(tt)
                    tile_s1(j, i, tt)
                    if nxt is not None:
                        nxt.stageA2(tt)
                if nxt is not None:
                    nxt.stageB()
                rstd_block(j)
                for tt in range(4):
                    tile_s2a(j, tt)
                    if nxt is not None:
                        nxt.stageC(tt)
                        merge((jj + 1) % 2, tt)
                for tt in range(4):
                    tile_s2b(j, tt)
                S.coll(lambda j=j: nc.gpsimd.collective_compute(
                    "AllGather", ALU.bypass, replica_groups=groups,
                    ins=[v_s[j].opt()], outs=[ccvd[j].ap().opt()]),
                    reads=[b_vs[j]], writes=[b_ccvd[j]])
            DMA(lambda: nc.sync.dma_start(out=ccLs.ap()[:, :], in_=logit[:, 0:32, :].rearrange("p g e -> p (g e)")),
                r=[b_logit], w=[b_ccLs])
            S.coll(lambda: nc.gpsimd.collective_compute("AllGather", ALU.bypass, replica_groups=groups,
                                                        ins=[ccLs.ap().opt()], outs=[ccLd.ap().opt()]),
                   reads=[b_ccLs], writes=[b_ccLd])
            S.barrier()
            ck(4)

        lru_st.close()
        with ExitStack() as st:
            aff = sb(st, "aff", [128, 64, NE], F32); b_aff = Buf("aff")
            mx = sb(st, "mx", [128, 64], F32); b_mx = Buf("mx")
            affT = sb(st, "affT", [NE, T], F32); b_affT = Buf("affT")
            junk = sb(st, "junk", [NE, T], F32); b_junk = Buf("junk")
            pa = psum(st, "pa", [NE, 2048], F32); b_pa = Buf("pa")
            lgp = sb(st, "lgp", [128, 32, NE], F32); b_lgp = Buf("lgp")
            DMA(lambda: nc.gpsimd.indirect_dma_start(
                out=lgp[:].rearrange("p g e -> p (g e)"), out_offset=None, in_=ccLd.ap()[:, :],
                in_offset=bass.IndirectOffsetOnAxis(ap=pidx_t[:, 36:37], axis=0),
                bounds_check=reg_c1, oob_is_err=False), r=[b_pidxt, b_ccLd], w=[b_lgp], q="pool")
            vts = [sb(st, "mvts%d" % i, [128, 513], I32) for i in range(4)]
            b_vts = [Buf("vts%d" % i) for i in range(4)]
            for gp in range(32):
                vt, b_vt = vts[gp % 4], b_vts[gp % 4]
                DMA(lambda gp=gp, vt=vt: nc.gpsimd.indirect_dma_start(
                    out=vt[:, :], out_offset=None, in_=ccvd[gp // 4].ap()[:, :],
                    in_offset=bass.IndirectOffsetOnAxis(ap=pidx_t[:, gp % 4:gp % 4 + 1], axis=0),
                    bounds_check=reg_cc, oob_is_err=False),
                    r=[b_pidxt, b_ccvd[gp // 4]], w=[b_vt], q="pool")
                G(lambda vt=vt: nc.gpsimd.tensor_scalar(out=vt[:, 512:513], in0=vt[:, 512:513],
                                                        scalar1=float(TO), scalar2=None, op0=ALU.add),
                  r=[b_vt], w=[b_vt])
                DMA(lambda gp=gp, vt=vt: nc.sync.dma_start(out=vp_s[gp * 128:(gp + 1) * 128, :], in_=vt[:]),
                    r=[b_vt], w=[b_vp[gp]])
            V(lambda: nc.vector.tensor_copy(out=logit[:, 32:64, 0:NEL], in_=lgp[:, :, NEL:NE]), r=[b_lgp], w=[b_logit])
            V(lambda: nc.vector.tensor_copy(out=logit[:, 32:64, NEL:NE], in_=lgp[:, :, 0:NEL]), r=[b_lgp], w=[b_logit])
            V(lambda: nc.vector.tensor_reduce(out=mx[:], in_=logit[:], axis=AX.X, op=ALU.max), r=[b_logit], w=[b_mx])
            V(lambda: nc.vector.tensor_tensor(out=aff[:], in0=logit[:], in1=mx[:].unsqueeze(2).to_broadcast([128, 64, NE]),
                                              op=ALU.subtract), r=[b_logit, b_mx], w=[b_aff])
            A(lambda: nc.scalar.activation(out=aff[:], in_=aff[:], func=AF.Exp), r=[b_aff], w=[b_aff])
            V(lambda: nc.vector.tensor_reduce(out=mx[:], in_=aff[:], axis=AX.X, op=ALU.add), r=[b_aff], w=[b_mx])
            V(lambda: nc.vector.reciprocal(out=mx[:], in_=mx[:]), r=[b_mx], w=[b_mx])
            V(lambda: nc.vector.tensor_tensor(out=aff[:], in0=aff[:], in1=mx[:].unsqueeze(2).to_broadcast([128, 64, NE]),
                                              op=ALU.mult), r=[b_aff, b_mx], w=[b_aff])
            DMA(lambda: nc.sync.dma_start(out=aff_s[0:T, :].rearrange("(g p) e -> p g e", p=128), in_=aff[:, :, :]),
                r=[b_aff], w=[b_affs])
            for q in range(4):
                for g in range(16):
                    P(lambda q=q, g=g: nc.tensor.transpose(out=pa[:, g * 128:(g + 1) * 128], in_=aff[:, q * 16 + g, :],
                                                           identity=identf[:]), r=[b_aff, b_identf], w=[b_pa])
                A(lambda q=q: nc.scalar.copy(out=affT[:, q * 2048:(q + 1) * 2048], in_=pa[:]), r=[b_pa], w=[b_affT])
            thr = sb(st, "thr", [NE, 1], F32); b_thr = Buf("thr")
            thb = sb(st, "thb", [128, NEL], F32); b_thb = Buf("thb")
            cdb = sb(st, "cdb", [128, NEL], F32); b_cdb = Buf("cdb")
            cpb = sb(st, "cpb", [128, NEL], F32); b_cpb = Buf("cpb")
            msk = sb(st, "msk", [128, 64, NEL], F32); b_msk = Buf("msk")
            ones1 = sb(st, "ones1", [128, 128], F32); b_ones1 = Buf("ones1")
            pct = psum(st, "pct", [128, NEL], F32); b_pct = Buf("pct")
            V(lambda: nc.vector.memset(ones1[:], 1.0), w=[b_ones1])
            V(lambda: nc.vector.memset(thb[:], 0.0), w=[b_thb])
            for it in range(1, 25):
                step = 2.0 ** (-it)
                V(lambda step=step: nc.vector.tensor_scalar(out=cdb[:], in0=thb[:], scalar1=step, scalar2=None,
                                                            op0=ALU.add), r=[b_thb], w=[b_cdb])
                V(lambda: nc.vector.tensor_tensor(out=msk[:], in0=aff[:, :, 0:NEL],
                                                  in1=cdb[:].unsqueeze(1).to_broadcast([128, 64, NEL]), op=ALU.is_ge),
                  r=[b_aff, b_cdb], w=[b_msk])
                V(lambda: nc.vector.tensor_reduce(out=cpb[:], in_=msk[:].rearrange("p g e -> p e g"), axis=AX.X,
                                                  op=ALU.add), r=[b_msk], w=[b_cpb])
                P(lambda: nc.tensor.matmul(pct[:], lhsT=ones1[:], rhs=cpb[:], start=True, stop=True),
                  r=[b_ones1, b_cpb], w=[b_pct])
                V(lambda step=step: nc.vector.tensor_scalar(out=cpb[:], in0=pct[:], scalar1=float(CAP) - 0.5,
                                                            scalar2=step, op0=ALU.is_ge, op1=ALU.mult),
                  r=[b_pct], w=[b_cpb])
                V(lambda: nc.vector.tensor_tensor(out=thb[:], in0=thb[:], in1=cpb[:], op=ALU.add),
                  r=[b_thb, b_cpb], w=[b_thb])
            pth = psum(st, "pth", [NEL, 128], F32); b_pth = Buf("pth")
            V(lambda: nc.vector.memset(thr[:], 2.0), w=[b_thr])
            P(lambda: nc.tensor.transpose(out=pth[:], in_=thb[:], identity=identf[:]), r=[b_thb, b_identf], w=[b_pth])
            V(lambda: nc.vector.tensor_copy(out=thr[0:NEL, :], in_=pth[:, 0:1]), r=[b_pth], w=[b_thr])
            if dbg:
                dd = ddbg("thr", [NE, 1])
                DMA(lambda: nc.sync.dma_start(out=dd[:, :], in_=thr[:]), r=[b_thr])
                dd2 = ddbg("aff", [128, 64, NE])
                DMA(lambda: nc.sync.dma_start(out=dd2[:, :, :], in_=aff[:]), r=[b_aff])
            mk = junk[:, :]
            pst = sb(st, "pst", [NE, T], F32)
            ps_ = pst[:, :]
            zr = sb(st, "zr", [NE, T], F32); b_zr = Buf("zr")
            V(lambda: nc.vector.memset(zr[:], 0.0), w=[b_zr])
            V(lambda: nc.vector.tensor_scalar(out=mk, in0=affT[:, :], scalar1=thr[:, 0:1], scalar2=None,
                                              op0=ALU.is_ge), r=[b_affT, b_thr], w=[b_junk])
            V(lambda: nc.vector.tensor_tensor_scan(out=ps_, data0=mk, data1=zr[:], initial=0.0, op0=ALU.add,
                                                   op1=ALU.add), r=[b_junk, b_zr], w=[b_junk])
            V(lambda: nc.vector.tensor_scalar(out=mk, in0=mk, scalar1=-float(T), scalar2=float(T) - 1.0,
                                              op0=ALU.mult, op1=ALU.add), r=[b_junk], w=[b_junk])
            V(lambda: nc.vector.tensor_tensor(out=ps_, in0=ps_, in1=mk, op=ALU.add), r=[b_junk], w=[b_junk])
            pidx = psum(st, "pidx", [128, NT, NE], F32); b_pidx = Buf("pidx")
            for g in range(NT):
                P(lambda g=g: nc.tensor.transpose(out=pidx[:, g, :], in_=pst[:, g * 128:(g + 1) * 128],
                                                  identity=identf[0:NE, 0:NE]), r=[b_junk, b_identf], w=[b_pidx])
            V(lambda: nc.vector.tensor_copy(out=idx[:], in_=pidx[:]), r=[b_pidx], w=[b_idx])
            if dbg:
                dd = ddbg("idx", [128, NT, NE], I32)
                DMA(lambda: nc.sync.dma_start(out=dd[:, :, :], in_=idx[:]), r=[b_idx])
            S.barrier()
            ck(5)

        reg_cap = nc.gpsimd.to_reg(CAP - 1)
        reg_tot = nc.gpsimd.to_reg(T + TRASH - 1)
        with ExitStack() as st:
            xe = [sb(st, "xe%d" % i, [128, 4, 513], I32) for i in range(1)] * 2
            b_xet = [Buf("xet%d" % i) for i in range(1)] * 2
            toks = [sb(st, "toks%d" % i, [128, 8, 1], I32) for i in range(2)]
            b_toks = [Buf("toks%d" % i) for i in range(2)]
            m5b = sb(st, "m5b", [128, D], F32); b_m5b = Buf("m5b")
            gsel = [sb(st, "gsel%d" % i, [128, 8, NE], F32) for i in range(2)]
            b_gsel = [Buf("gsel%d" % i) for i in range(2)]
            xeT = sb(st, "xeT", [128, 8, CAP], BF16); b_xeT = Buf("xeT")
            hT = sb(st, "hT", [128, NF, CAP], BF16); b_hT = [Buf("hT%d" % i) for i in range(2)]
            wgf = [sb(st, "wgf%d" % i, [128, 8, 128], F32) for i in range(2)]
            b_wgf = [Buf("wgf%d" % i) for i in range(2)]
            wuf = [sb(st, "wuf%d" % i, [128, 8, 128], F32) for i in range(2)]
            b_wuf = [Buf("wuf%d" % i) for i in range(2)]
            wgb = [sb(st, "wgb%d" % i, [128, 8, 128], BF16) for i in range(2)]
            b_wgb = [Buf("wgb%d" % i) for i in range(2)]
            wub = [sb(st, "wub%d" % i, [128, 8, 128], BF16) for i in range(2)]
            b_wub = [Buf("wub%d" % i) for i in range(2)]
            wdf = [sb(st, "wdf%d" % i, [128, D], F32) for i in range(2)]
            b_wdf = [Buf("wdf%d" % i) for i in range(2)]
            wdb = sb(st, "wdb", [128, NF, D], BF16); b_wdb = Buf("wdb")
            hg = [sb(st, "hg%d" % i, [128, 512], F32) for i in range(2)]
            b_hg = [Buf("hg%d" % i) for i in range(2)]
            yst = [sb(st, "yst%d" % i, [128, D], F32) for i in range(1)] * 2
            b_yst = [Buf("yst%d" % i) for i in range(1)] * 2
            pgu = [(psum(st, "pg%d" % i, [128, 512], F32), Buf("pg%d" % i),
                    psum(st, "pu%d" % i, [128, 512], F32), Buf("pu%d" % i)) for i in range(2)]
            pdn = [(psum(st, "pd%d" % i, [128, 512], F32), Buf("pd%d" % i)) for i in range(3)]
            pxT = psum(st, "pxT", [128, 8, 128], BF16); b_pxT = Buf("pxT")
            make_bc(pdn[0:2], m5b, b_m5b, 0, 5 * D, "copy")
            NVT = 8
            vts = [sb(st, "vts%d" % i, [128, 513], I32) for i in range(NVT)]
            b_vts = [Buf("vts%d" % i) for i in range(NVT)]
            sc_rr = [0]

            def v_load(g, slot, q):
                vt, b_vt = vts[slot], b_vts[slot]
                eng = {"sp": nc.sync, "pool": nc.gpsimd, "act": nc.scalar}[q]
                if g < 32:
                    DMA(lambda: eng.dma_start(out=vt[:], in_=v_s[g // 4][(g % 4) * 128:(g % 4 + 1) * 128, :]),
                        r=[b_vs[g // 4]], w=[b_vt], q=q)
                else:
                    DMA(lambda: eng.dma_start(out=vt[:], in_=vp_s[(g - 32) * 128:(g - 31) * 128, :]),
                        r=[b_vp[g - 32]], w=[b_vt], q=q)

            def v_scatter(e, g, slot):
                vt, b_vt = vts[slot], b_vts[slot]
                DMA(lambda: nc.gpsimd.indirect_dma_start(
                    out=xe_s[e][:, :], out_offset=bass.IndirectOffsetOnAxis(ap=idx[:, g, e:e + 1], axis=0),
                    in_=vt[:, :], in_offset=None, bounds_check=reg_cap, oob_is_err=False),
                    r=[b_idx, b_vt, b_xe[e]], w=[b_xe[e]], q="pool")

            def scatter_stream(e, q):
                DEPTH = 4
                base = sc_rr[0]
                for g in range(DEPTH):
                    v_load(g, (base + g) % NVT, q)
                for g in range(NT):
                    v_scatter(e, g, (base + g) % NVT)
                    if e == 0:
                        v_scatter(1, g, (base + g) % NVT)
                    if g + DEPTH < NT:
                        v_load(g + DEPTH, (base + g + DEPTH) % NVT, q)
                sc_rr[0] = (base + NT) % NVT

            scatter_stream(0, "sp")
            cast_rr = 0
            gu_rr = 0
            dn_rr = 0
            for e in range(NEL):
                xi = e % 2
                xet, b_x = xe[xi], b_xet[xi]
                tk, b_tk = toks[xi], b_toks[xi]
                for hf_ in range(2):
                    DMA(lambda e=e, xet=xet, hf_=hf_: nc.sync.dma_start(
                        out=xet[:], in_=xe_s[e].rearrange("(b p) w -> p b w", p=128)[:, hf_ * 4:(hf_ + 1) * 4, :]),
                        r=[b_xe[e]], w=[b_x])
                    V(lambda xet=xet, tk=tk, hf_=hf_: nc.vector.tensor_copy(out=tk[:, hf_ * 4:(hf_ + 1) * 4, :],
                                                                           in_=xet[:, :, 512:513]),
                      r=[b_x], w=[b_tk])
                    for bl in range(4):
                        blk = hf_ * 4 + bl
                        for k in range(8):
                            P(lambda bl=bl, k=k, xet=xet: nc.tensor.transpose(
                                out=pxT[:, k, :], in_=xet[:, bl, 0:512].bitcast(BF16)[:, k * 128:(k + 1) * 128],
                                identity=ident[:]), r=[b_x, b_ident], w=[b_pxT])
                        if blk % 2 == 0:
                            A(lambda blk=blk: nc.scalar.copy(out=xeT[:, :, blk * 128:(blk + 1) * 128], in_=pxT[:]),
                              r=[b_pxT], w=[b_xeT])
                        else:
                            V(lambda blk=blk: nc.vector.tensor_copy(out=xeT[:, :, blk * 128:(blk + 1) * 128],
                                                                    in_=pxT[:]), r=[b_pxT], w=[b_xeT])
                for blk in range(8):
                    DMA(lambda blk=blk, tk=tk, xi=xi: nc.gpsimd.indirect_dma_start(
                        out=gsel[xi][:, blk, :], out_offset=None, in_=aff_s[:, :],
                        in_offset=bass.IndirectOffsetOnAxis(ap=tk[:, blk, :], axis=0),
                        bounds_check=reg_tot, oob_is_err=False),
                        r=[b_tk, b_affs], w=[b_gsel[xi]], q="pool")
                for f in range(NF):
                    wi = f % 2
                    DMA(lambda e=e, f=f, wi=wi: nc.sync.dma_start(
                        out=wgf[wi][:], in_=wg_d[e, f]),
                        w=[b_wgf[wi]])
                    DMA(lambda e=e, f=f, wi=wi: nc.sync.dma_start(
                        out=wuf[wi][:], in_=wu_d[e, f]),
                        w=[b_wuf[wi]])
                    A(lambda wi=wi: nc.scalar.copy(out=wgb[wi][:], in_=wgf[wi][:]), r=[b_wgf[wi]], w=[b_wgb[wi]])
                    if f % 4 < 2:
                        V(lambda wi=wi: nc.vector.tensor_copy(out=wub[wi][:], in_=wuf[wi][:]),
                          r=[b_wuf[wi]], w=[b_wub[wi]])
                    else:
                        A(lambda wi=wi: nc.scalar.copy(out=wub[wi][:], in_=wuf[wi][:]),
                          r=[b_wuf[wi]], w=[b_wub[wi]])
                    for ns in range(2):
                        pg, b_pg, pu, b_pu = pgu[gu_rr % 2]
                        hgi, b_hgi = hg[gu_rr % 2], b_hg[gu_rr % 2]
                        gu_rr += 1
                        for k in range(8):
                            P(lambda k=k, wi=wi, ns=ns, pg=pg: nc.tensor.matmul(
                                pg[:], lhsT=wgb[wi][:, k, :],
                                rhs=xeT[:, k, ns * 512:(ns + 1) * 512], start=(k == 0), stop=(k == 7)),
                              r=[b_wgb[wi], b_xeT], w=[b_pg])
                        for k in range(8):
                            P(lambda k=k, wi=wi, ns=ns, pu=pu: nc.tensor.matmul(
                                pu[:], lhsT=wub[wi][:, k, :],
                                rhs=xeT[:, k, ns * 512:(ns + 1) * 512], start=(k == 0), stop=(k == 7)),
                              r=[b_wub[wi], b_xeT], w=[b_pu])
                        A(lambda pg=pg, hgi=hgi: nc.scalar.activation(out=hgi[:], in_=pg[:], func=AF.Silu),
                          r=[b_pg], w=[b_hgi])
                        V(lambda pu=pu, hgi=hgi, f=f, ns=ns: nc.vector.tensor_tensor(
                            out=hT[:, f, ns * 512:(ns + 1) * 512], in0=pu[:], in1=hgi[:], op=ALU.mult),
                          r=[b_pu, b_hgi], w=[b_hT[ns]])
                    DMA(lambda e=e, f=f, wi=wi: nc.sync.dma_start(
                        out=wdf[wi][:], in_=wd_d[e, f * 128:(f + 1) * 128, :]), w=[b_wdf[wi]])
                    V(lambda f=f, wi=wi: nc.vector.tensor_copy(out=wdb[:, f, :], in_=wdf[wi][:]),
                      r=[b_wdf[wi]], w=[b_wdb])
                    if e % 2 == 1 and e + 1 < NEL:
                        for g in range(f * 3, min(NT, f * 3 + 3)):
                            slot = sc_rr[0] % NVT
                            sc_rr[0] += 1
                            v_load(g, slot, "act")
                            v_scatter(e + 1, g, slot)
                            if e + 2 < NEL:
                                v_scatter(e + 2, g, slot)

                for blk in range(8):
                    yi = blk % 2
                    ys, b_ys = yst[yi], b_yst[yi]
                    for n in range(2):
                        pd, b_pd = pdn[dn_rr % 3]
                        dn_rr += 1
                        for f in range(NF):
                            P(lambda f=f, blk=blk, n=n, pd=pd: nc.tensor.matmul(
                                pd[:], lhsT=hT[:, f, blk * 128:(blk + 1) * 128], rhs=wdb[:, f, n * 512:(n + 1) * 512],
                                start=(f == 0), stop=(f == NF - 1)),
                              r=[b_hT[blk // 4], b_wdb], w=[b_pd])
                        sl = slice(n * 512, (n + 1) * 512)
                        V(lambda pd=pd, ys=ys, sl=sl, blk=blk, xi=xi, e=e: nc.vector.scalar_tensor_tensor(
                            out=ys[:, sl], in0=pd[:], scalar=gsel[xi][:, blk, e:e + 1], in1=m5b[:, sl],
                            op0=ALU.mult, op1=ALU.mult), r=[b_pd, b_gsel[xi], b_m5b], w=[b_ys])
                    DMA(lambda blk=blk, ys=ys, tk=tk: nc.gpsimd.indirect_dma_start(
                        out=acc_s[:, :], out_offset=bass.IndirectOffsetOnAxis(ap=tk[:, blk, :], axis=0),
                        in_=ys[:], in_offset=None, bounds_check=reg_tot, oob_is_err=True,
                        compute_op=ALU.add), r=[b_ys, b_tk, b_acc], w=[b_acc], q="pool")
            S.barrier()

        for k in range(NCC):
            DMA(lambda k=k: nc.sync.dma_start(
                out=ccsrc[k].ap().rearrange("p (q n) -> p q n", q=4),
                in_=acc_s[TO + k * CCR:TO + (k + 1) * CCR, :].rearrange("(q p) n -> p q n", p=128)),
                r=[b_acc], w=[b_ccsrc[k]])
        for k in range(NCC):
            S.coll(lambda k=k: nc.gpsimd.collective_compute(
                "AllGather", ALU.bypass, replica_groups=groups,
                ins=[ccsrc[k].ap().opt()], outs=[ccdst[k].ap().opt()]),
                reads=[b_ccsrc[k]], writes=[b_ccdst[k]])

        with ExitStack() as st:
            rp3, b_rp3 = load_rowp(st, 3)
            xt4 = [sb(st, "fx%d" % i, [128, 4, D], F32) for i in range(2)]
            b_xt4 = [Buf("fx%d" % i) for i in range(2)]
            pr4 = [sb(st, "fp%d" % i, [128, 4, D], F32) for i in range(2)]
            b_pr4 = [Buf("fp%d" % i) for i in range(2)]
            ot4 = [sb(st, "fo%d" % i, [128, 4, D], F32) for i in range(2)]
            b_ot4 = [Buf("fo%d" % i) for i in range(2)]
            tmp = sb(st, "ftmp", [128, D], BF16); b_tmp = Buf("ftmp")
            b_out = Buf("out")
            for k in range(NCC):
                i = k % 2
                DMA(lambda k=k, i=i: nc.sync.dma_start(
                    out=xt4[i][:], in_=acc_s[k * CCR:(k + 1) * CCR, :].rearrange("(q p) n -> p q n", p=128)),
                    r=[b_acc], w=[b_xt4[i]])
                DMA(lambda k=k, i=i: nc.gpsimd.indirect_dma_start(
                    out=pr4[i][:].rearrange("p q n -> p (q n)"), out_offset=None, in_=ccdst[k].ap()[:, :],
                    in_offset=bass.IndirectOffsetOnAxis(ap=pidx_t[:, 36:37], axis=0),
                    bounds_check=reg_c1, oob_is_err=False),
                    r=[b_pidxt, b_ccdst[k]], w=[b_pr4[i]], q="pool")
                V(lambda i=i: nc.vector.tensor_tensor(out=xt4[i][:], in0=xt4[i][:], in1=pr4[i][:], op=ALU.add),
                  r=[b_xt4[i], b_pr4[i]], w=[b_xt4[i]])
                for q_ in range(4):
                    g = k * 4 + q_
                    A(lambda i=i, q_=q_, g=g: nc.scalar.activation(out=tmp[:], in_=xt4[i][:, q_, :], func=AF.Square,
                                                                   accum_out=ssq[:, g:g + 1]),
                      r=[b_xt4[i]], w=[b_tmp, b_ssq])
                g0 = k * 4
                A(lambda g0=g0: nc.scalar.activation(out=rstd[:, g0:g0 + 4], in_=ssq[:, g0:g0 + 4], func=AF.Sqrt,
                                                     scale=1.0 / D, bias=epsr[:, 0:1]), r=[b_ssq, b_eps], w=[b_rstd])
                V(lambda g0=g0: nc.vector.reciprocal(out=rstd[:, g0:g0 + 4], in_=rstd[:, g0:g0 + 4]),
                  r=[b_rstd], w=[b_rstd])
                for q_ in range(4):
                    g = k * 4 + q_
                    V(lambda i=i, q_=q_, g=g: nc.vector.scalar_tensor_tensor(
                        out=ot4[i][:, q_, :], in0=xt4[i][:, q_, :], scalar=rstd[:, g:g + 1], in1=rp3[:],
                        op0=ALU.mult, op1=ALU.mult), r=[b_xt4[i], b_rstd, b_rp3], w=[b_ot4[i]])
                DMA(lambda k=k, i=i: nc.sync.dma_start(
                    out=out_d[k * CCR:(k + 1) * CCR, :].rearrange("(q p) n -> p q n", p=128), in_=ot4[i][:]),
                    r=[b_ot4[i]], w=[b_out])
            S.barrier()
        big.close()
        print("kernel build: insts", S.n_inst, "waits", S.n_wait)
    except _Stop:
        pass
    return nc, dbg_out


_WCACHE = {}


def _expert_layout(inp, grp):
    key = (id(inp["exp_w_gate"]), grp)
    if key not in _WCACHE:
        es_ = slice(grp * NEL, (grp + 1) * NEL)
        out = []
        for name in ("exp_w_gate", "exp_w_up"):
            w = inp[name][0][es_]
            w = w.reshape(NEL, 8, 128, NF, 128).transpose(0, 3, 2, 1, 4)
            out.append(np.ascontiguousarray(w, dtype=np.float32))
        _WCACHE[key] = out
    return _WCACHE[key]


def _prep_core(c, inp):
    b, flip = c // 2, c % 2
    f32 = np.float32
    xb = inp["x"][b]
    ctxb = inp["ctx"][b]
    conv_w = inp["conv_dw_w"][0]
    if flip:
        xb = xb[::-1]
        ctxb = ctxb[::-1]
        conv_w = conv_w[::-1]
    dirs = [1, 0] if flip else [0, 1]
    cc = np.zeros((128, 8, 2), f32)
    cc[:, :, 0] = inp["c"][b].reshape(8, 128).T
    cc[:, :, 1] = inp["c_ctx"].reshape(8, 128).T
    rowp = np.zeros((128, 4, D), f32)
    rowp[:, 0, :] = inp["norm1_g"][0][None, :]
    rowp[:, 1, :] = inp["norm2_g"][0][None, :]
    rowp[:, 2, :] = inp["b_out"][0][None, :]
    rowp[:, 3, :] = inp["final_norm_g"][None, :]
    colp = np.zeros((128, NCOL), f32)
    colp[:, C_BIN:C_BIN + 16] = inp["b_in"][0].reshape(16, 128).T
    colp[:, C_CB:C_CB + 4] = inp["conv_dw_b"][0].reshape(4, 128).T
    colp[:, C_LG:C_LG + 4] = inp["conv_ln_g"][0].reshape(4, 128).T
    colp[:, C_LB:C_LB + 4] = inp["conv_ln_b"][0].reshape(4, 128).T
    colp[:, C_CW:C_CW + 124] = conv_w.reshape(31, 4, 128).transpose(2, 1, 0).reshape(128, 124)
    gw = np.zeros((128, 2, 2, 4, 128), f32)
    for dp, d in enumerate(dirs):
        colp[:, C_LCB + dp * 4:C_LCB + dp * 4 + 4] = inp["lru_conv_b"][0, d].reshape(4, 128).T
        colp[:, C_BA + dp * 4:C_BA + dp * 4 + 4] = inp["lru_ba"][0, d].reshape(4, 128).T
        colp[:, C_BI + dp * 4:C_BI + dp * 4 + 4] = inp["lru_bi"][0, d].reshape(4, 128).T
        colp[:, C_LAM + dp * 4:C_LAM + dp * 4 + 4] = inp["lru_lambda"][0, d].reshape(4, 128).T
        colp[:, C_LCW + dp * 16:C_LCW + dp * 16 + 16] = \
            inp["lru_conv_w"][0, d].reshape(4, 4, 128).transpose(2, 1, 0).reshape(128, 16)
        for gi, key in enumerate(("lru_wa", "lru_wi")):
            w = inp[key][0, d]
            for cch in range(4):
                for hh in range(2):
                    gw[hh * 64:(hh + 1) * 64, dp, gi, cch, hh * 64:(hh + 1) * 64] = w[2 * cch + hh]
    grp = c % 2
    perm = list(range(grp * NEL, (grp + 1) * NEL)) + list(range((1 - grp) * NEL, (2 - grp) * NEL))
    rw = inp["router_w"][0][:, perm]
    pidx = np.zeros((128, 40), np.int32)
    rp = 1 - grp
    ar = np.arange(128)
    for q in range(4):
        pidx[:, q] = rp * CCR + q * 128 + ar
    for g in range(32):
        pidx[:, 4 + g] = rp * TO + g * 128 + ar
    pidx[:, 36] = rp * 128 + ar
    es_ = slice(grp * NEL, (grp + 1) * NEL)
    return {
        "pidx": pidx,
        "xb": np.ascontiguousarray(xb, dtype=f32),
        "ctxb": np.ascontiguousarray(ctxb, dtype=f32),
        "cc": cc,
        "ada_w": np.ascontiguousarray(inp["ada_w"][0], dtype=f32),
        "ada_b2": np.ascontiguousarray(np.broadcast_to(inp["ada_b"][0][None, :], (2, 6 * D)), dtype=f32),
        "rowp": rowp,
        "colp": colp,
        "w_in": np.ascontiguousarray(inp["w_in"][0], dtype=f32),
        "w_out": np.ascontiguousarray(inp["w_out"][0], dtype=f32),
        "router_w": np.ascontiguousarray(rw.reshape(8, 128, NE).transpose(1, 0, 2), dtype=f32),
        "gw": gw,
        "wg": _expert_layout(inp, grp)[0],
        "wu": _expert_layout(inp, grp)[1],
        "wd": np.ascontiguousarray(inp["exp_w_down"][0][es_], dtype=f32),
    }


def kernel(**inputs):
    inp = {k: np.asarray(v) for k, v in inputs.items()}
    nc, _ = build_nc(DEBUG)
    in_maps = [_prep_core(c, inp) for c in range(8)]
    res = run_bass_kernel_spmd(nc, in_maps, core_ids=list(range(8)))
    _WCACHE.clear()
    out = np.zeros((4, T, D), np.float32)
    for c in range(8):
        b, flip = c // 2, c % 2
        o = np.asarray(res.results[c]["out"], dtype=np.float32)
        if flip:
            out[b, TO:] = o[::-1]
        else:
            out[b, :TO] = o
    return out
```

```python
import numpy as np
from contextlib import ExitStack
import concourse.bass as bass
import concourse.mybir as mybir
from concourse.bass_utils import run_bass_kernel_spmd

F32 = mybir.dt.float32
BF16 = mybir.dt.bfloat16
I32 = mybir.dt.int32
ALU = mybir.AluOpType
AF = mybir.ActivationFunctionType
AX = mybir.AxisListType

T = 8192
TO = 4096
D = 1024
NE = 16
CAP = 1024
DE = 2816
NF = DE // 128
L = 512
NB = T // L
CTX = 256
TRASH = CAP
NEL = 8
NT = T // 128
CCR = 512
NCC = TO // CCR
NB1 = 10
NBO = 8
NCOL = 16 + 4 + 4 + 4 + 124 + 8 + 8 + 8 + 8 + 32
C_BIN, C_CB, C_LG, C_LB, C_CW = 0, 16, 20, 24, 28
C_LCB, C_BA, C_BI, C_LAM, C_LCW = 152, 160, 168, 176, 184

DEBUG = False


class Buf:
    __slots__ = ("name", "w", "r")

    def __init__(self, name):
        self.name = name
        self.w = None
        self.r = {}


class Sched:
    N_DMA_SEMS = 8

    def __init__(self, nc, es):
        self.nc = nc
        self.engs = {"pe": nc.tensor, "act": nc.scalar, "dve": nc.vector,
                     "pool": nc.gpsimd, "sp": nc.sync}
        self.sems = {}
        self.count = {}
        self.seen = {k: {} for k in self.engs}
        for k in self.engs:
            self.sems[k] = es.enter_context(nc.semaphore("s_" + k))
            self.count[k] = 0
        self.dma_sems = {}
        self.dma_rr = {}
        for q in ("sp", "pool", "act"):
            lst = []
            for i in range(self.N_DMA_SEMS):
                key = "d_%s%d" % (q, i)
                self.sems[key] = es.enter_context(nc.semaphore(key))
                self.count[key] = 0
                lst.append(key)
            self.dma_sems[q] = lst
            self.dma_rr[q] = 0
        self.sems["cc"] = es.enter_context(nc.semaphore("s_cc"))
        self.count["cc"] = 0
        self.n_inst = 0
        self.n_wait = 0

    def coll(self, fn, reads=(), writes=()):
        self._deps("pool", reads, writes)
        ins = fn()
        self.count["cc"] += 1
        ins.then_inc(self.sems["cc"])
        self._mark("cc", self.count["cc"], reads, writes)
        self.n_inst += 1
        return ins

    def _wait(self, eng, key, val):
        if val <= 0:
            return
        s = self.seen[eng]
        if s.get(key, 0) >= val:
            return
        self.engs[eng].wait_ge(self.sems[key], val)
        s[key] = val
        self.n_wait += 1

    def _deps(self, eng, reads, writes):
        for b in reads:
            if b.w is not None:
                self._wait(eng, b.w[0], b.w[1])
        for b in writes:
            if b.w is not None and not (eng == "pe" and b.w[0] == "pe"):
                self._wait(eng, b.w[0], b.w[1])
            for k, v in b.r.items():
                self._wait(eng, k, v)

    def _mark(self, key, val, reads, writes):
        for b in reads:
            if b.r.get(key, 0) < val:
                b.r[key] = val
        for b in writes:
            b.w = (key, val)
            b.r = {}

    def op(self, eng, fn, reads=(), writes=()):
        self._deps(eng, reads, writes)
        ins = fn()
        self.count[eng] += 1
        ins.then_inc(self.sems[eng], 1)
        self._mark(eng, self.count[eng], reads, writes)
        self.n_inst += 1
        return ins

    def dma(self, q, fn, reads=(), writes=()):
        key = self.dma_sems[q][self.dma_rr[q] % self.N_DMA_SEMS]
        self.dma_rr[q] += 1
        self._wait(q, key, self.count[key])
        self._deps(q, reads, writes)
        ins = fn()
        self.count[key] += 16
        ins.then_inc(self.sems[key], 16)
        self._mark(key, self.count[key], reads, writes)
        self.n_inst += 1
        return ins

    def barrier(self):
        for e in self.engs:
            for k in self.sems:
                if k != e:
                    self._wait(e, k, self.count[k])
            self._wait(e, e, self.count[e])


class _Stop(Exception):
    pass


def build_nc(dbg=False, stop_after=99):
    nc = bass.Bass("TRN2", target_bir_lowering=False)

    def din(name, shape, dt=F32):
        return nc.dram_tensor(name, list(shape), dt, kind="ExternalInput").ap()

    def dscr(name, shape, dt):
        return nc.dram_tensor(name, list(shape), dt, kind="Internal").ap()

    x_d = din("xb", [T, D])
    ctx_d = din("ctxb", [CTX, D])
    cc_d = din("cc", [128, 8, 2])
    adaw_d = din("ada_w", [D, 6 * D])
    adab_d = din("ada_b2", [2, 6 * D])
    rowp_d = din("rowp", [128, 4, D])
    colp_d = din("colp", [128, NCOL])
    win_d = din("w_in", [D, 2048])
    wout_d = din("w_out", [D, D])
    rw_d = din("router_w", [128, 8, NE])
    gw_d = din("gw", [128, 2, 2, 4, 128])
    if stop_after >= 6:
        wg_d = din("wg", [NEL, NF, 128, 8, 128])
        wu_d = din("wu", [NEL, NF, 128, 8, 128])
        wd_d = din("wd", [NEL, DE, D])
    pidx_d = din("pidx", [128, 40], I32)
    out_d = nc.dram_tensor("out", [TO, D], F32, kind="ExternalOutput").ap()

    lx_s = dscr("lx_s", [4, 128, T + 6], BF16)
    glg_s = dscr("glg_s", [4, 128, T], BF16)
    hf_s = dscr("hf_s", [4, 128, T], BF16)
    cx_s = dscr("cx_s", [4, 128, T], BF16)
    acc_s = dscr("acc_s", [T + TRASH, D], F32)
    aff_s = dscr("aff_s", [T + TRASH, NE], F32)
    ccsrc = [nc.dram_tensor("ccsrc%d" % k, [128, 4 * D], F32, kind="Internal") for k in range(NCC)]
    ccdst = [nc.dram_tensor("ccdst%d" % k, [256, 4 * D], F32, kind="Internal") for k in range(NCC)]
    xe_s = [dscr("xe_s%d" % e, [CAP, 513], I32) for e in range(NEL)]
    mod_s = dscr("mod_s", [2, 6 * D], F32)
    v_s = [dscr("v_s%d" % k, [CCR, 513], I32) for k in range(NCC)]
    ccvd = [nc.dram_tensor("ccvd%d" % k, [2 * CCR, 513], I32, kind="Internal") for k in range(NCC)]
    ccLs = nc.dram_tensor("ccLs", [128, 32 * NE], F32, kind="Internal")
    ccLd = nc.dram_tensor("ccLd", [256, 32 * NE], F32, kind="Internal")
    vp_s = dscr("vp_s", [TO, 513], I32)
    cc1s = nc.dram_tensor("cc1s", [128, 4], F32, kind="Internal")
    cc1d = nc.dram_tensor("cc1d", [256, 4], F32, kind="Internal")

    dbg_out = {}

    def ddbg(name, shape, dt=F32):
        if dbg:
            dbg_out[name] = nc.dram_tensor("dbg_" + name, list(shape), dt, kind="ExternalOutput").ap()
            return dbg_out[name]
        return None

    es = ExitStack()
    try:
      with es:
        S = Sched(nc, es)

        def ck(k):
            if stop_after == k:
                S.barrier()
                print("kernel build (stop %d): insts" % k, S.n_inst, "waits", S.n_wait)
                raise _Stop()

        uid = [0]

        def sb(st, name, shape, dt):
            uid[0] += 1
            return st.enter_context(nc.sbuf_tensor("s%d_%s" % (uid[0], name), list(shape), dt))

        def psum(st, name, shape, dt):
            uid[0] += 1
            return st.enter_context(nc.psum_tensor("p%d_%s" % (uid[0], name), list(shape), dt))

        V = lambda fn, r=(), w=(): S.op("dve", fn, r, w)
        A = lambda fn, r=(), w=(): S.op("act", fn, r, w)
        P = lambda fn, r=(), w=(): S.op("pe", fn, r, w)
        G = lambda fn, r=(), w=(): S.op("pool", fn, r, w)
        DMA = lambda fn, r=(), w=(), q="sp": S.dma(q, fn, r, w)

        b_lx = [Buf("lx_s%d" % j) for j in range(NB + 1)]
        b_glg = [Buf("glg%d" % j) for j in range(NB)]
        b_hf = [Buf("hf%d" % j) for j in range(NB)]
        b_cx = [Buf("cx%d" % j) for j in range(NB)]
        b_acc = Buf("acc")
        b_affs = Buf("affs")
        b_xe = [Buf("xe%d" % e) for e in range(NEL)]
        b_ccsrc = [Buf("ccsrc%d" % k) for k in range(NCC)]
        b_ccdst = [Buf("ccdst%d" % k) for k in range(NCC)]
        b_mods = Buf("mod_s")
        b_vs = [Buf("v_s%d" % k) for k in range(NCC)]
        b_ccvd = [Buf("ccvd%d" % k) for k in range(NCC)]
        b_vp = [Buf("vp%d" % g) for g in range(32)]
        b_ccLs = Buf("ccLs"); b_ccLd = Buf("ccLd"); b_cc1s = Buf("cc1s"); b_cc1d = Buf("cc1d")

        ident = sb(es, "ident", [128, 128], BF16); b_ident = Buf("ident")
        identf = sb(es, "identf", [128, 128], F32); b_identf = Buf("identf")
        colp = sb(es, "colp", [128, NCOL], F32); b_colp = Buf("colp")
        cst = sb(es, "cst", [128, 4], F32); b_cst = Buf("cst")
        sel = sb(es, "sel", [2, 2, 128], F32); b_sel = Buf("sel")
        ssq = sb(es, "ssq", [128, 64], F32); b_ssq = Buf("ssq")
        rstd = sb(es, "rstd", [128, 64], F32); b_rstd = Buf("rstd")
        mr_t = (sb(es, "mr", [2, D], F32), Buf("mr"))
        pidx_t = sb(es, "pidx_t", [128, 40], I32); b_pidxt = Buf("pidx_t")
        DMA(lambda: nc.sync.dma_start(out=pidx_t[:], in_=pidx_d[:, :]), w=[b_pidxt])
        groups = [[0, 1], [2, 3], [4, 5], [6, 7]]
        reg_c1 = nc.gpsimd.to_reg(255)
        reg_cL = nc.gpsimd.to_reg(2 * TO - 1)
        reg_cc = nc.gpsimd.to_reg(2 * CCR - 1)
        big = es.enter_context(ExitStack())
        logit = sb(big, "logit", [128, 64, NE], F32); b_logit = Buf("logit")
        idx = sb(big, "idx", [128, NT, NE], I32); b_idx = Buf("idx")
        lru_st = es.enter_context(ExitStack())
        clam = sb(lru_st, "clam", [128, 2, 8], F32); b_clam = Buf("clam")
        carry = sb(lru_st, "carry", [128, 8], F32); b_carry = Buf("carry")
        hbias = sb(lru_st, "hbias", [128, 16], F32); b_hbias = Buf("hbias")
        dg4 = sb(lru_st, "dg4", [128, 32, 128], BF16); b_dg4 = Buf("dg4")
        gwb = sb(lru_st, "gwb", [128, 16, 128], BF16); b_gwb = Buf("gwb")
        scb_h = [None, None]
        shb_h = [None, None]
        b_scb = Buf("scb")
        b_shb = Buf("shb")

        G(lambda: nc.gpsimd.memset(identf[:], 0.0), w=[b_identf])
        G(lambda: nc.gpsimd.affine_select(out=identf[:], in_=identf[:], pattern=[[-1, 128]],
                                          compare_op=ALU.not_equal, fill=1.0, base=0,
                                          channel_multiplier=1), r=[b_identf], w=[b_identf])
        V(lambda: nc.vector.tensor_copy(out=ident[:], in_=identf[:]), r=[b_identf], w=[b_ident])
        DMA(lambda: nc.sync.dma_start(out=colp[:], in_=colp_d[:, :]), w=[b_colp])
        V(lambda: nc.vector.memset(cst[:, 0:1], 1e-6), w=[b_cst])
        V(lambda: nc.vector.memset(cst[:, 1:2], 1e-5), w=[b_cst])
        V(lambda: nc.vector.memset(cst[:, 2:3], 1.0), w=[b_cst])
        V(lambda: nc.vector.memset(cst[:, 3:4], 0.25), w=[b_cst])

        def load_rowp(st, r):
            t = sb(st, "rowp%d" % r, [128, D], F32)
            b = Buf("rowp%d" % r)
            DMA(lambda: nc.sync.dma_start(out=t[:], in_=rowp_d[:, r, :]), w=[b])
            return t, b

        with ExitStack() as st:
            zt = sb(st, "zt", [128, 4, 3], BF16); b_zt = Buf("zt")
            V(lambda: nc.vector.memset(zt[:], 0.0), w=[b_zt])
            DMA(lambda: nc.sync.dma_start(out=lx_s.rearrange("c p t -> p c t")[:, :, 0:3], in_=zt[:]),
                r=[b_zt], w=[b_lx[0]])
            DMA(lambda: nc.sync.dma_start(out=lx_s.rearrange("c p t -> p c t")[:, :, T + 3:T + 6], in_=zt[:]),
                r=[b_zt], w=[b_lx[NB]])
            S.barrier()

        with ExitStack() as st:
            modrow = sb(st, "modrow", [2, 6 * D], F32); b_mod = Buf("modrow")
            cc = sb(st, "cc", [128, 8, 2], F32); b_cc = Buf("cc")
            cs = sb(st, "cs", [128, 8, 2], F32); b_cs = Buf("cs")
            adab = sb(st, "adab", [2, 6 * D], F32); b_adab = Buf("adab")
            seli = sb(st, "seli", [2, 2, 128], I32); b_seli = Buf("seli")
            awt = [sb(st, "awt%d" % i, [128, 8, 512], F32) for i in range(2)]
            b_awt = [Buf("awt%d" % i) for i in range(2)]
            pm = [psum(st, "pm%d" % i, [128, 512], F32) for i in range(2)]
            b_pm = [Buf("pm%d" % i) for i in range(2)]
            lam_t = sb(st, "lam_t", [128, 8], F32); b_lam = Buf("lam_t")

            DMA(lambda: nc.sync.dma_start(out=cc[:], in_=cc_d[:, :, :]), w=[b_cc])
            DMA(lambda: nc.sync.dma_start(out=adab[:], in_=adab_d[:, :]), w=[b_adab])
            A(lambda: nc.scalar.activation(out=cs[:], in_=cc[:], func=AF.Silu), r=[b_cc], w=[b_cs])
            G(lambda: nc.gpsimd.iota(seli[:], pattern=[[-1, 2], [0, 128]], base=0, channel_multiplier=1),
              w=[b_seli])
            V(lambda: nc.vector.tensor_single_scalar(out=sel[:], in_=seli[:], scalar=0.0, op=ALU.is_equal),
              r=[b_seli], w=[b_sel])
            A(lambda: nc.scalar.activation(out=lam_t[:], in_=colp[:, C_LAM:C_LAM + 8], func=AF.Exp, scale=-1.0),
              r=[b_colp], w=[b_lam])
            A(lambda: nc.scalar.activation(out=lam_t[:], in_=lam_t[:], func=AF.Ln, bias=cst[:, 2:3], scale=1.0),
              r=[b_lam, b_cst], w=[b_lam])
            V(lambda: nc.vector.tensor_scalar(out=clam[:, 0, :], in0=lam_t[:], scalar1=-4.0, scalar2=None,
                                              op0=ALU.mult), r=[b_lam], w=[b_clam])
            V(lambda: nc.vector.tensor_scalar(out=clam[:, 1, :], in0=lam_t[:], scalar1=-8.0, scalar2=None,
                                              op0=ALU.mult), r=[b_lam], w=[b_clam])
            V(lambda: nc.vector.tensor_scalar(out=hbias[:], in0=colp[:, C_BA:C_BA + 16], scalar1=0.5, scalar2=None,
                                              op0=ALU.mult), r=[b_colp], w=[b_hbias])
            for n in range(12):
                i = n % 2
                DMA(lambda n=n, i=i: nc.sync.dma_start(
                    out=awt[i][:], in_=adaw_d.rearrange("(k p) n -> p k n", p=128)[:, :, n * 512:(n + 1) * 512]),
                    w=[b_awt[i]])
                for k in range(8):
                    P(lambda i=i, k=k: nc.tensor.matmul(pm[i][0:2, :], lhsT=cs[:, k, :], rhs=awt[i][:, k, :],
                                                        start=(k == 0), stop=(k == 7)),
                      r=[b_cs, b_awt[i]], w=[b_pm[i]])
                V(lambda n=n, i=i: nc.vector.tensor_tensor(out=modrow[:, n * 512:(n + 1) * 512], in0=pm[i][0:2, :],
                                                           in1=adab[:, n * 512:(n + 1) * 512], op=ALU.add),
                  r=[b_pm[i], b_adab], w=[b_mod])

            for d in range(2):
                for c in range(4):
                    for k in range(4):
                        col = C_LCW + d * 16 + c * 4 + k
                        V(lambda d=d, c=c, k=k, col=col: nc.vector.tensor_scalar(
                            out=dg4[:, d * 16 + c * 4 + k, :], in0=identf[:], scalar1=colp[:, col:col + 1],
                            scalar2=None, op0=ALU.mult), r=[b_identf, b_colp], w=[b_dg4])
            gwf = sb(st, "gwf", [128, 16, 128], F32); b_gwf = Buf("gwf")
            DMA(lambda: nc.sync.dma_start(out=gwf[:], in_=gw_d.rearrange("p d g c q -> p (d g c) q")), w=[b_gwf])
            V(lambda: nc.vector.tensor_copy(out=gwb[:], in_=gwf[:]), r=[b_gwf], w=[b_gwb])
            DMA(lambda: nc.sync.dma_start(out=mod_s[:, :], in_=modrow[:]), r=[b_mod], w=[b_mods])
            if dbg:
                dd = ddbg("modrow", [2, 6 * D])
                DMA(lambda: nc.sync.dma_start(out=dd[:, :], in_=modrow[:]), r=[b_mod])
            S.barrier()
            ck(0)

        def make_bc(st_ps, dst, b_dst, j, off, mode, other=None, b_other=None):
            mr, b_mr = mr_t
            DMA(lambda: nc.sync.dma_start(out=mr[:], in_=mod_s[:, off:off + D]), r=[b_mods], w=[b_mr])
            for n in range(2):
                pb, b_pb = st_ps[n]
                P(lambda n=n, pb=pb: nc.tensor.matmul(pb[:], lhsT=sel[:, j, :],
                                                      rhs=mr[:, n * 512:(n + 1) * 512],
                                                      start=True, stop=True),
                  r=[b_sel, b_mr], w=[b_pb])
                sl = slice(n * 512, (n + 1) * 512)
                if mode == "copy":
                    A(lambda pb=pb, sl=sl: nc.scalar.copy(out=dst[:, sl], in_=pb[:]), r=[b_pb], w=[b_dst])
                elif mode == "1p_mul":
                    V(lambda pb=pb, sl=sl: nc.vector.scalar_tensor_tensor(
                        out=dst[:, sl], in0=pb[:], scalar=1.0, in1=other[:, sl], op0=ALU.add, op1=ALU.mult),
                      r=[b_pb, b_other], w=[b_dst])
                elif mode == "mul":
                    V(lambda pb=pb, sl=sl: nc.vector.tensor_tensor(out=dst[:, sl], in0=pb[:], in1=other[:, sl],
                                                                   op=ALU.mult), r=[b_pb, b_other], w=[b_dst])

        def norm_tile(src_ap, xt, b_xt, gidx, tmp, b_tmp, ub, b_ub, pT, b_pT, uT, b_uT, col0, q="sp"):
            norm_pre(src_ap, xt, b_xt, gidx, tmp, b_tmp, ub, b_ub)
            norm_post(ub, b_ub, pT, b_pT, uT, b_uT, col0)

        def norm_pre(src_ap, xt, b_xt, gidx, tmp, b_tmp, ub, b_ub, q="sp"):
            DMA(lambda: nc.sync.dma_start(out=xt[:], in_=src_ap), w=[b_xt], q=q)
            A(lambda: nc.scalar.activation(out=tmp[:], in_=xt[:], func=AF.Square, accum_out=ssq[:, gidx:gidx + 1]),
              r=[b_xt], w=[b_tmp, b_ssq])
            A(lambda: nc.scalar.activation(out=rstd[:, gidx:gidx + 1], in_=ssq[:, gidx:gidx + 1], func=AF.Sqrt,
                                           scale=1.0 / D, bias=epsr[:, 0:1]), r=[b_ssq, b_eps], w=[b_rstd])
            V(lambda: nc.vector.reciprocal(out=rstd[:, gidx:gidx + 1], in_=rstd[:, gidx:gidx + 1]),
              r=[b_rstd], w=[b_rstd])
            V(lambda: nc.vector.scalar_tensor_tensor(out=tmp[:], in0=xt[:], scalar=rstd[:, gidx:gidx + 1],
                                                     in1=scb_h[0][:], op0=ALU.mult, op1=ALU.mult),
              r=[b_xt, b_rstd, b_scb], w=[b_tmp])
            V(lambda: nc.vector.tensor_tensor(out=ub[:], in0=tmp[:], in1=shb_h[0][:], op=ALU.add),
              r=[b_tmp, b_shb], w=[b_ub])

        def norm_post(ub, b_ub, pT, b_pT, uT, b_uT, col0):
            for k in range(8):
                P(lambda k=k: nc.tensor.transpose(out=pT[:, k, :], in_=ub[:, k * 128:(k + 1) * 128],
                                                  identity=ident[:]), r=[b_ub, b_ident], w=[b_pT])
            A(lambda: nc.scalar.copy(out=uT[:, :, col0:col0 + 128], in_=pT[:]), r=[b_pT], w=[b_uT])

        epsr, b_eps = cst, b_cst

        class Lru:
            def __init__(self, t, lxw, b_lxw, Lb, d, hout, b_hout, pxs, b_pxs):
                self.t, self.lxw, self.b_lxw, self.Lb, self.d = t, lxw, b_lxw, Lb, d
                self.hout, self.b_hout, self.pxs, self.b_pxs = hout, b_hout, pxs, b_pxs

            def stageA(self, c):
                self.stageA1(c)
                self.stageA2(c)

            def stageA1(self, c):
                t, lxw, b_lxw, Lb, d = self.t, self.lxw, self.b_lxw, self.Lb, self.d
                dc = d * 4 + c
                ns = len(self.pxs) // 3
                o3 = (c % ns) * 3
                px, b_px = self.pxs[o3 + 0], self.b_pxs[o3 + 0]
                pr, b_pr = self.pxs[o3 + 1], self.b_pxs[o3 + 1]
                pi, b_pi = self.pxs[o3 + 2], self.b_pxs[o3 + 2]
                xcf, b_xcf = t["xcf"][c % 2]
                xcb, b_xcb = t["xcb"][c % 2]
                tr, b_tr = t["tr"][c % 2]
                ti, b_ti = t["ti"][c % 2]
                a4, b_a4 = t["a4"]; om4, b_om4 = t["om4"]; ix4, b_ix4 = t["ix4"]
                for k in range(4):
                    o = k if d == 0 else 3 - k
                    P(lambda k=k, o=o: nc.tensor.matmul(px[:, 0:Lb], lhsT=dg4[:, d * 16 + c * 4 + k, :],
                                                        rhs=lxw[:, c, o:o + Lb], start=(k == 0), stop=(k == 3)),
                      r=[b_dg4, b_lxw], w=[b_px])
                A(lambda: nc.scalar.activation(out=xcf[:, 0:Lb], in_=px[:, 0:Lb], func=AF.Identity,
                                               bias=colp[:, C_LCB + dc:C_LCB + dc + 1], scale=1.0),
                  r=[b_px, b_colp], w=[b_xcf])
                V(lambda: nc.vector.tensor_copy(out=xcb[:, 0:Lb], in_=xcf[:, 0:Lb]), r=[b_xcf], w=[b_xcb])

            def stageA2(self, c):
                t, lxw, b_lxw, Lb, d = self.t, self.lxw, self.b_lxw, self.Lb, self.d
                dc = d * 4 + c
                ns = len(self.pxs) // 3
                o3 = (c % ns) * 3
                pr, b_pr = self.pxs[o3 + 1], self.b_pxs[o3 + 1]
                pi, b_pi = self.pxs[o3 + 2], self.b_pxs[o3 + 2]
                xcf, b_xcf = t["xcf"][c % 2]
                xcb, b_xcb = t["xcb"][c % 2]
                tr, b_tr = t["tr"][c % 2]
                ti, b_ti = t["ti"][c % 2]
                a4, b_a4 = t["a4"]; om4, b_om4 = t["om4"]; ix4, b_ix4 = t["ix4"]
                P(lambda: nc.tensor.matmul(pr[:, 0:Lb], lhsT=gwb[:, d * 8 + 0 * 4 + c, :], rhs=xcb[:, 0:Lb],
                                           start=True, stop=True), r=[b_gwb, b_xcb], w=[b_pr])
                P(lambda: nc.tensor.matmul(pi[:, 0:Lb], lhsT=gwb[:, d * 8 + 1 * 4 + c, :], rhs=xcb[:, 0:Lb],
                                           start=True, stop=True), r=[b_gwb, b_xcb], w=[b_pi])
                A(lambda: nc.scalar.activation(out=tr[:, 0:Lb], in_=pr[:, 0:Lb], func=AF.Tanh,
                                               bias=hbias[:, dc:dc + 1], scale=0.5), r=[b_pr, b_hbias], w=[b_tr])
                A(lambda: nc.scalar.activation(out=ti[:, 0:Lb], in_=pi[:, 0:Lb], func=AF.Tanh,
                                               bias=hbias[:, 8 + dc:8 + dc + 1], scale=0.5),
                  r=[b_pi, b_hbias], w=[b_ti])
                A(lambda: nc.scalar.activation(out=a4[:, c, 0:Lb], in_=tr[:, 0:Lb], func=AF.Exp,
                                               scale=clam[:, 0, dc:dc + 1], bias=clam[:, 0, dc:dc + 1]),
                  r=[b_tr, b_clam], w=[b_a4[c]])
                A(lambda: nc.scalar.activation(out=om4[:, c, 0:Lb], in_=tr[:, 0:Lb], func=AF.Exp,
                                               scale=clam[:, 1, dc:dc + 1], bias=clam[:, 1, dc:dc + 1]),
                  r=[b_tr, b_clam], w=[b_om4[c]])
                V(lambda: nc.vector.scalar_tensor_tensor(out=ix4[:, c, 0:Lb], in0=ti[:, 0:Lb], scalar=1.0,
                                                         in1=xcf[:, 0:Lb], op0=ALU.add, op1=ALU.mult),
                  r=[b_ti, b_xcf], w=[b_ix4[c]])

            def stageB(self):
                t, Lb = self.t, self.Lb
                om4, b_om4 = t["om4"]
                A(lambda: nc.scalar.activation(out=om4[:, :, 0:Lb], in_=om4[:, :, 0:Lb], func=AF.Sqrt,
                                               scale=-0.25, bias=cst[:, 3:4]), r=list(b_om4) + [b_cst], w=list(b_om4))

            def stageC(self, c):
                t, Lb, d, hout, b_hout = self.t, self.Lb, self.d, self.hout, self.b_hout
                dc = d * 4 + c
                a4, b_a4 = t["a4"]; om4, b_om4 = t["om4"]; ix4, b_ix4 = t["ix4"]
                V(lambda: nc.vector.tensor_tensor(out=ix4[:, c, 0:Lb], in0=ix4[:, c, 0:Lb], in1=om4[:, c, 0:Lb],
                                                  op=ALU.mult), r=[b_ix4[c], b_om4[c]], w=[b_ix4[c]])
                if d == 0:
                    V(lambda: nc.vector.tensor_tensor_scan(out=hout[:, c, 0:Lb], data0=a4[:, c, 0:Lb],
                                                           data1=ix4[:, c, 0:Lb], initial=carry[:, dc:dc + 1],
                                                           op0=ALU.mult, op1=ALU.add),
                      r=[b_a4[c], b_ix4[c], b_carry], w=[b_hout])
                    V(lambda: nc.vector.tensor_copy(out=carry[:, dc:dc + 1], in_=hout[:, c, Lb - 1:Lb]),
                      r=[b_hout], w=[b_carry])
                else:
                    V(lambda: nc.vector.tensor_tensor_scan(out=hout[:, c, 0:Lb][:, ::-1],
                                                           data0=a4[:, c, 0:Lb][:, ::-1],
                                                           data1=ix4[:, c, 0:Lb][:, ::-1],
                                                           initial=carry[:, dc:dc + 1], op0=ALU.mult, op1=ALU.add),
                      r=[b_a4[c], b_ix4[c], b_carry], w=[b_hout])
                    V(lambda: nc.vector.tensor_copy(out=carry[:, dc:dc + 1], in_=hout[:, c, 0:1]),
                      r=[b_hout], w=[b_carry])

            def run(self):
                for c in range(4):
                    self.stageA(c)
                self.stageB()
                for c in range(4):
                    self.stageC(c)

        def lru_block(st_t, lxw, b_lxw, Lb, d, hout, b_hout, pxs, b_pxs):
            Lru(st_t, lxw, b_lxw, Lb, d, hout, b_hout, pxs, b_pxs).run()

        def lru_tiles(st, W=L):
            t = {}
            for nm, dt in (("xcf", F32), ("xcb", BF16), ("tr", F32), ("ti", F32)):
                t[nm] = [(sb(st, "lr_%s%d" % (nm, i), [128, W], dt), Buf("lr_%s%d" % (nm, i))) for i in range(2)]
            for nm in ("a4", "om4", "ix4"):
                t[nm] = (sb(st, "lr_" + nm, [128, 4, W], F32), [Buf("lr_%s%d" % (nm, c)) for c in range(4)])
            return t

        glu_st = es.enter_context(ExitStack())
        glu = sb(glu_st, "glu", [128, 4, T], BF16); b_glu = [Buf("glu%d" % j) for j in range(NB)]
        with ExitStack() as st:
            winb = sb(st, "winb", [128, 8, 2048], BF16); b_winb = Buf("winb")
            scb = sb(st, "scb", [128, D], F32); shb = sb(st, "shb", [128, D], F32)
            scb_h[0] = scb; shb_h[0] = shb
            rp0, b_rp0 = load_rowp(st, 0)
            st2 = ExitStack()
            wst = [sb(st2, "wst%d" % i, [128, 8, 256], F32) for i in range(2)]
            b_wst = [Buf("wst%d" % i) for i in range(2)]
            for n in range(8):
                i = n % 2
                DMA(lambda n=n, i=i: nc.sync.dma_start(
                    out=wst[i][:], in_=win_d.rearrange("(k p) n -> p k n", p=128)[:, :, n * 256:(n + 1) * 256]),
                    w=[b_wst[i]])
                if n % 2 == 0:
                    V(lambda n=n, i=i: nc.vector.tensor_copy(out=winb[:, :, n * 256:(n + 1) * 256], in_=wst[i][:]),
                      r=[b_wst[i]], w=[b_winb])
                else:
                    A(lambda n=n, i=i: nc.scalar.copy(out=winb[:, :, n * 256:(n + 1) * 256], in_=wst[i][:]),
                      r=[b_wst[i]], w=[b_winb])
            S.barrier()
            st2.close()
            xts = [sb(st, "xt%d" % i, [128, D], F32) for i in range(2)]
            b_xts = [Buf("xt%d" % i) for i in range(2)]
            tmp = sb(st, "tmp", [128, D], F32); b_tmp = Buf("tmp")
            ubs = [sb(st, "ub%d" % i, [128, D], BF16) for i in range(4)]
            b_ubs = [Buf("ub%d" % i) for i in range(4)]
            uTs = [sb(st, "uT%d" % i, [128, 8, L], BF16) for i in range(2)]
            b_uTs = [Buf("uT%d" % i) for i in range(2)]
            pT = psum(st, "pT", [128, 8, 128], BF16); b_pT = Buf("pT")
            pbs = [(psum(st, "pb%d" % i, [128, 512], F32), Buf("pb%d" % i)) for i in range(4)]
            pxs = [psum(st, "px%d" % i, [128, 512], F32) for i in range(3)]
            b_pxs = [Buf("px%d" % i) for i in range(3)]
            stc = ExitStack()
            lt = lru_tiles(stc, CTX)

            make_bc(pbs[0:2], scb, b_scb, 1, 1 * D, "1p_mul", rp0, b_rp0)
            make_bc(pbs[2:4], shb, b_shb, 1, 0 * D, "copy")
            clx = sb(stc, "clx", [128, 4, CTX + 6], BF16); b_clx = Buf("clx")
            chh = sb(stc, "chh", [128, 4, CTX], F32); b_chh = Buf("chh")
            V(lambda: nc.vector.memset(clx[:], 0.0), w=[b_clx])
            V(lambda: nc.vector.memset(carry[:], 0.0), w=[b_carry])
            for tt in range(2):
                norm_tile(ctx_d[tt * 128:(tt + 1) * 128, :], xts[tt], b_xts[tt], tt, tmp, b_tmp, ubs[tt], b_ubs[tt],
                          pT, b_pT, uTs[0], b_uTs[0], tt * 128)
            for c in range(4):
                m = 8 + c
                pb, b_pb = pbs[c % 4]
                for k in range(8):
                    P(lambda k=k, m=m, pb=pb: nc.tensor.matmul(pb[:, 0:CTX], lhsT=winb[:, k, m * 128:(m + 1) * 128],
                                                               rhs=uTs[0][:, k, 0:CTX], start=(k == 0), stop=(k == 7)),
                      r=[b_winb, b_uTs[0]], w=[b_pb])
                A(lambda c=c, m=m, pb=pb: nc.scalar.activation(out=clx[:, c, 3:3 + CTX], in_=pb[:, 0:CTX],
                                                               func=AF.Identity, bias=colp[:, m:m + 1], scale=1.0),
                  r=[b_pb, b_colp], w=[b_clx])
            lru_block(lt, clx[:, :, 0:CTX + 3], b_clx, CTX, 0, chh, b_chh, pxs, b_pxs)
            lru_block(lt, clx[:, :, 3:CTX + 6], b_clx, CTX, 1, chh, b_chh, pxs, b_pxs)
            if dbg:
                dd = ddbg("carry0", [128, 8])
                DMA(lambda: nc.sync.dma_start(out=dd[:, :], in_=carry[:]), r=[b_carry])
            S.barrier()
            stc.close()
            ck(0.5)
            sig = [sb(st, "sig%d" % i, [128, L], F32) for i in range(2)]
            b_sig = [Buf("sig%d" % i) for i in range(2)]
            lxst = [sb(st, "lxst%d" % i, [128, 4, L], BF16) for i in range(2)]
            b_lxst = [Buf("lxst%d" % i) for i in range(2)]
            ggst = [sb(st, "ggst%d" % i, [128, 4, L], BF16) for i in range(2)]
            b_ggst = [Buf("ggst%d" % i) for i in range(2)]

            make_bc(pbs[0:2], scb, b_scb, 0, 1 * D, "1p_mul", rp0, b_rp0)
            make_bc(pbs[2:4], shb, b_shb, 0, 0 * D, "copy")

            def npre(jb, tt):
                g = jb * 4 + tt
                norm_pre(x_d[g * 128:(g + 1) * 128, :], xts[g % 2], b_xts[g % 2], g, tmp, b_tmp, ubs[tt], b_ubs[tt])

            def npost(jb, tt):
                norm_post(ubs[tt], b_ubs[tt], pT, b_pT, uTs[jb % 2], b_uTs[jb % 2], tt * 128)

            for tt in range(4):
                npre(0, tt)
                npost(0, tt)
            for j in range(NB1):
                uT, b_uT = uTs[j % 2], b_uTs[j % 2]
                order = [4, 0, 5, 1, 6, 2, 7, 3, 8, 9, 10, 11, 12, 13, 14, 15]
                for qi, m in enumerate(order):
                    if qi % 4 == 0 and j + 1 < NB1:
                        npre(j + 1, qi // 4)
                        if qi >= 4:
                            npost(j + 1, qi // 4 - 1)
                    pb, b_pb = pbs[qi % 4]
                    for k in range(8):
                        P(lambda k=k, m=m, pb=pb: nc.tensor.matmul(pb[:], lhsT=winb[:, k, m * 128:(m + 1) * 128],
                                                                   rhs=uT[:, k, :], start=(k == 0), stop=(k == 7)),
                          r=[b_winb, b_uT], w=[b_pb])
                    if 4 <= m < 8:
                        sg, b_sg = sig[m % 2], b_sig[m % 2]
                        A(lambda m=m, pb=pb, sg=sg: nc.scalar.activation(out=sg[:], in_=pb[:], func=AF.Sigmoid,
                                                                         bias=colp[:, m:m + 1], scale=1.0),
                          r=[b_pb, b_colp], w=[b_sg])
                    elif m < 4:
                        sg, b_sg = sig[m % 2], b_sig[m % 2]
                        V(lambda m=m, pb=pb, sg=sg: nc.vector.scalar_tensor_tensor(
                            out=glu[:, m, j * L:(j + 1) * L], in0=pb[:], scalar=colp[:, m:m + 1], in1=sg[:],
                            op0=ALU.add, op1=ALU.mult), r=[b_pb, b_colp, b_sg], w=[b_glu[j]])
                    elif m < 12:
                        A(lambda m=m, pb=pb: nc.scalar.activation(out=lxst[j % 2][:, m - 8, :], in_=pb[:],
                                                                  func=AF.Identity, bias=colp[:, m:m + 1], scale=1.0),
                          r=[b_pb, b_colp], w=[b_lxst[j % 2]])
                    else:
                        A(lambda m=m, pb=pb: nc.scalar.activation(out=ggst[j % 2][:, m - 12, :], in_=pb[:],
                                                                  func=AF.Gelu_apprx_tanh, bias=colp[:, m:m + 1],
                                                                  scale=1.0),
                          r=[b_pb, b_colp], w=[b_ggst[j % 2]])
                if j + 1 < NB1:
                    npost(j + 1, 3)
                DMA(lambda j=j: nc.sync.dma_start(
                    out=lx_s.rearrange("c p t -> p c t")[:, :, 3 + j * L:3 + (j + 1) * L], in_=lxst[j % 2][:]),
                    r=[b_lxst[j % 2]], w=[b_lx[j]])
                DMA(lambda j=j: nc.sync.dma_start(
                    out=glg_s.rearrange("c p t -> p c t")[:, :, j * L:(j + 1) * L], in_=ggst[j % 2][:]),
                    r=[b_ggst[j % 2]], w=[b_glg[j]])
            if dbg:
                dd = ddbg("glu", [128, 4, T], BF16)
                DMA(lambda: nc.sync.dma_start(out=dd[:, :, :], in_=glu[:]), r=b_glu)
            S.barrier()
            ck(1)

        with ExitStack() as st:
            lt = lru_tiles(st)
            pxs = [psum(st, "px%d" % i, [128, 512], F32) for i in range(6)]
            b_pxs = [Buf("px%d" % i) for i in range(6)]
            lxw = [sb(st, "lxw%d" % i, [128, 4, L + 3], BF16) for i in range(2)]
            b_lxw = [Buf("lxw%d" % i) for i in range(2)]
            hh = [sb(st, "hh%d" % i, [128, 4, L], F32) for i in range(2)]
            b_hh = [Buf("hh%d" % i) for i in range(2)]
            hb16 = [sb(st, "hb16%d" % i, [128, 4, L], BF16) for i in range(2)]
            b_hb16 = [Buf("hb16%d" % i) for i in range(2)]
            def mk_lru(j):
                i = j % 2
                deps = [b_lx[j]] + ([b_lx[j - 1]] if j > 0 else [b_lx[0]])
                DMA(lambda: nc.sync.dma_start(
                    out=lxw[i][:], in_=lx_s.rearrange("c p t -> p c t")[:, :, j * L:j * L + L + 3]),
                    r=deps, w=[b_lxw[i]])
                return Lru(lt, lxw[i], b_lxw[i], L, 0, hh[i], b_hh[i], pxs, b_pxs)

            cur = mk_lru(0)
            for c in range(4):
                cur.stageA(c)
            cur.stageB()
            for j in range(NBO):
                i = j % 2
                nxt = mk_lru(j + 1) if j + 1 < NBO else None
                for c in range(4):
                    cur.stageC(c)
                    if nxt is not None:
                        nxt.stageA(c)
                if nxt is not None:
                    nxt.stageB()
                G(lambda i=i: nc.gpsimd.tensor_copy(out=hb16[i][:], in_=hh[i][:]), r=[b_hh[i]], w=[b_hb16[i]])
                DMA(lambda j=j, i=i: nc.sync.dma_start(
                    out=hf_s.rearrange("c p t -> p c t")[:, :, j * L:(j + 1) * L], in_=hb16[i][:]),
                    r=[b_hb16[i]], w=[b_hf[j]])
                cur = nxt
            DMA(lambda: nc.sync.dma_start(out=cc1s.ap()[:, :], in_=carry[:, 0:4]), r=[b_carry], w=[b_cc1s])
            S.coll(lambda: nc.gpsimd.collective_compute("AllGather", ALU.bypass, replica_groups=groups,
                                                        ins=[cc1s.ap().opt()], outs=[cc1d.ap().opt()]),
                   reads=[b_cc1s], writes=[b_cc1d])
            DMA(lambda: nc.gpsimd.indirect_dma_start(
                out=carry[:, 4:8], out_offset=None, in_=cc1d.ap()[:, :],
                in_offset=bass.IndirectOffsetOnAxis(ap=pidx_t[:, 36:37], axis=0),
                bounds_check=reg_c1, oob_is_err=False), r=[b_pidxt, b_cc1d], w=[b_carry], q="pool")
            S.barrier()
            ck(2)

        with ExitStack() as st:
            dgc = sb(st, "dgc", [128, 124, 128], BF16); b_dgc = Buf("dgc")
            zt4 = sb(st, "zt4", [128, 2, D], F32); b_zt4 = Buf("zt4")
            V(lambda: nc.vector.memset(zt4[:], 0.0), w=[b_zt4])
            tid = sb(st, "tid", [128, 8, 1], I32); b_tid = Buf("tid")
            G(lambda: nc.gpsimd.iota(tid[:], pattern=[[128, 8], [0, 1]], base=T, channel_multiplier=1), w=[b_tid])
            for k in range(2 * NCC):
                DMA(lambda k=k: nc.sync.dma_start(
                    out=acc_s[TO + k * 256:TO + (k + 1) * 256, :].rearrange("(j p) n -> p j n", p=128), in_=zt4[:]),
                    r=[b_zt4], w=[b_acc])
            for e in range(NEL):
                DMA(lambda e=e: nc.sync.dma_start(out=xe_s[e].rearrange("(b p) w -> p b w", p=128)[:, :, 512:513],
                                                  in_=tid[:], allow_slow_non_contiguous=True),
                    r=[b_tid], w=[b_xe[e]])
            for c in range(4):
                for k in range(31):
                    col = C_CW + c * 31 + k
                    eng = V
                    if eng is V:
                        V(lambda c=c, k=k, col=col: nc.vector.tensor_scalar(
                            out=dgc[:, c * 31 + k, :], in0=identf[:], scalar1=colp[:, col:col + 1], scalar2=None,
                            op0=ALU.mult), r=[b_identf, b_colp], w=[b_dgc])
                    else:
                        G(lambda c=c, k=k, col=col: nc.gpsimd.tensor_scalar(
                            out=dgc[:, c * 31 + k, :], in0=identf[:], scalar1=colp[:, col:col + 1], scalar2=None,
                            op0=ALU.mult), r=[b_identf, b_colp], w=[b_dgc])
            onesf = sb(st, "onesf", [128, 128], F32); b_ones = Buf("onesf")
            V(lambda: nc.vector.memset(onesf[:], 1.0 / 512.0), w=[b_ones])
            pcs = [psum(st, "pc%d" % i, [128, 512], F32) for i in range(4)]
            b_pcs = [Buf("pc%d" % i) for i in range(4)]
            pmean = psum(st, "pmean", [128, 512], F32); b_pmean = Buf("pmean")
            pex2 = psum(st, "pex2", [128, 512], F32); b_pex2 = Buf("pex2")
            yf = [sb(st, "yf%d" % i, [128, 4, L], F32) for i in range(2)]
            b_yf = [Buf("yf%d" % i) for i in range(2)]
            y2 = sb(st, "y2", [128, 4, L], F32); b_y2 = Buf("y2")
            mean = sb(st, "mean", [128, L], F32); b_mean = Buf("mean")
            var = sb(st, "var", [128, L], F32); b_var = Buf("var")
            cxst = [sb(st, "cxst%d" % i, [128, 4, L], BF16) for i in range(2)]
            b_cxst = [Buf("cxst%d" % i) for i in range(2)]
            for j in range(NBO):
                i = j % 2
                r0 = j * 8
                for c in range(4):
                    mms = []
                    for k in [15] + [kk for kk in range(31) if kk != 15]:
                        s_ = k - 15
                        if c < 2:
                            g3 = glu[:, c, :].rearrange("p (r w) -> p r w", w=64)
                            p3 = pcs[c][:].rearrange("p (r w) -> p r w", w=64)
                            rhs = g3[:, r0:r0 + 8, max(0, s_):64 + min(0, s_)]
                            out = p3[:, :, max(0, -s_):64 - max(0, s_)]
                            jl, jh = j, j
                        else:
                            r_lo = max(r0, -s_)
                            r_hi = min(r0 + 8, 128 - s_)
                            if r_lo >= r_hi:
                                continue
                            rhs = glu[:, c, (r_lo + s_) * 64:(r_hi + s_) * 64]
                            out = pcs[c][:, (r_lo - r0) * 64:(r_hi - r0) * 64]
                            jl, jh = (r_lo + s_) // 8, (r_hi + s_ - 1) // 8
                        mms.append((k, rhs, out, jl, jh))
                    for q, (k, rhs, out, jl, jh) in enumerate(mms):
                        P(lambda c=c, k=k, rhs=rhs, out=out, q=q, n=len(mms): nc.tensor.matmul(
                            out, lhsT=dgc[:, c * 31 + k, :], rhs=rhs, start=(q == 0), stop=(q == n - 1)),
                          r=[b_dgc] + b_glu[jl:jh + 1], w=[b_pcs[c]])
                    A(lambda c=c, i=i: nc.scalar.activation(out=yf[i][:, c, :], in_=pcs[c][:], func=AF.Identity,
                                                            bias=colp[:, C_CB + c:C_CB + c + 1], scale=1.0),
                      r=[b_pcs[c], b_colp], w=[b_yf[i]])
                    G(lambda c=c, i=i: nc.gpsimd.tensor_tensor(out=y2[:, c, :], in0=yf[i][:, c, :], in1=yf[i][:, c, :],
                                                               op=ALU.mult), r=[b_yf[i]], w=[b_y2])
                for c in range(4):
                    P(lambda c=c, i=i: nc.tensor.matmul(pmean[:], lhsT=onesf[:], rhs=yf[i][:, c, :],
                                                        start=(c == 0), stop=(c == 3)),
                      r=[b_ones, b_yf[i]], w=[b_pmean])
                for c in range(4):
                    P(lambda c=c: nc.tensor.matmul(pex2[:], lhsT=onesf[:], rhs=y2[:, c, :],
                                                   start=(c == 0), stop=(c == 3)),
                      r=[b_ones, b_y2], w=[b_pex2])
                A(lambda: nc.scalar.copy(out=mean[:], in_=pmean[:]), r=[b_pmean], w=[b_mean])
                A(lambda: nc.scalar.activation(out=var[:], in_=pmean[:], func=AF.Square), r=[b_pmean], w=[b_var])
                V(lambda: nc.vector.tensor_tensor(out=var[:], in0=pex2[:], in1=var[:], op=ALU.subtract),
                  r=[b_pex2, b_var], w=[b_var])
                A(lambda: nc.scalar.activation(out=var[:], in_=var[:], func=AF.Sqrt, bias=epsr[:, 1:2], scale=1.0),
                  r=[b_var, b_eps], w=[b_var])
                V(lambda: nc.vector.reciprocal(out=var[:], in_=var[:]), r=[b_var], w=[b_var])
                for c in range(4):
                    V(lambda c=c, i=i: nc.vector.tensor_tensor(out=yf[i][:, c, :], in0=yf[i][:, c, :], in1=mean[:],
                                                               op=ALU.subtract), r=[b_yf[i], b_mean], w=[b_yf[i]])
                    V(lambda c=c, i=i: nc.vector.tensor_tensor(out=yf[i][:, c, :], in0=yf[i][:, c, :], in1=var[:],
                                                               op=ALU.mult), r=[b_yf[i], b_var], w=[b_yf[i]])
                    A(lambda c=c, i=i: nc.scalar.activation(out=cxst[i][:, c, :], in_=yf[i][:, c, :], func=AF.Silu,
                                                            bias=colp[:, C_LB + c:C_LB + c + 1],
                                                            scale=colp[:, C_LG + c:C_LG + c + 1]),
                      r=[b_yf[i], b_colp], w=[b_cxst[i]])
                DMA(lambda j=j, i=i: nc.sync.dma_start(
                    out=cx_s.rearrange("c p t -> p c t")[:, :, j * L:(j + 1) * L], in_=cxst[i][:]),
                    r=[b_cxst[i]], w=[b_cx[j]])
            S.barrier()
            ck(3)
        glu_st.close()

        with ExitStack() as st:
            woutb = sb(st, "woutb", [128, 8, D], BF16); b_woutb = Buf("woutb")
            scb = sb(st, "scb", [128, D], F32); shb = sb(st, "shb", [128, D], F32)
            scb_h[0] = scb; shb_h[0] = shb
            rp1, b_rp1 = load_rowp(st, 1)
            rp2, b_rp2 = load_rowp(st, 2)
            st2 = ExitStack()
            wst = [sb(st2, "wst%d" % i, [128, 8, 256], F32) for i in range(2)]
            b_wst = [Buf("wst%d" % i) for i in range(2)]
            for n in range(4):
                i = n % 2
                DMA(lambda n=n, i=i: nc.sync.dma_start(
                    out=wst[i][:], in_=wout_d.rearrange("(k p) n -> p k n", p=128)[:, :, n * 256:(n + 1) * 256]),
                    w=[b_wst[i]])
                V(lambda n=n, i=i: nc.vector.tensor_copy(out=woutb[:, :, n * 256:(n + 1) * 256], in_=wst[i][:]),
                  r=[b_wst[i]], w=[b_woutb])
            S.barrier()
            st2.close()
            rwt = sb(st, "rwt", [128, 8, NE], F32); b_rwt = Buf("rwt")
            DMA(lambda: nc.sync.dma_start(out=rwt[:], in_=rw_d[:, :, :]), w=[b_rwt])
            g1t = sb(st, "g1t", [128, D], F32); b_g1t = Buf("g1t")
            bob = sb(st, "bob", [128, D], F32); b_bob = Buf("bob")
            pbs = [(psum(st, "pb%d" % i, [128, 512], F32), Buf("pb%d" % i)) for i in range(2)]
            pxs = [psum(st, "px%d" % i, [128, 512], F32) for i in range(3)]
            b_pxs = [Buf("px%d" % i) for i in range(3)]
            pvT = psum(st, "pvT", [128, 8, 128], F32); b_pvT = Buf("pvT")
            plg = psum(st, "plg", [128, NE], F32); b_plg = Buf("plg")
            make_bc(pbs, g1t, b_g1t, 0, 2 * D, "copy")
            make_bc(pbs, bob, b_bob, 0, 2 * D, "mul", rp2, b_rp2)
            make_bc(pbs, scb, b_scb, 0, 4 * D, "1p_mul", rp1, b_rp1)
            make_bc(pbs, shb, b_shb, 0, 3 * D, "copy")
            vrow = [sb(st, "vrow%d" % i, [128, 513], I32) for i in range(2)]
            b_vrow = [Buf("vrow%d" % i) for i in range(2)]
            lt = lru_tiles(st)
            lxw = [sb(st, "lxw%d" % i, [128, 4, L + 3], BF16) for i in range(2)]
            b_lxw = [Buf("lxw%d" % i) for i in range(2)]
            hh = [sb(st, "hh%d" % i, [128, 4, L], F32) for i in range(2)]
            b_hh = [Buf("hh%d" % i) for i in range(2)]
            hfl = sb(st, "hfl", [128, 4, L], BF16); b_hfl = Buf("hfl")
            ggl = sb(st, "ggl", [128, 4, L], BF16); b_ggl = Buf("ggl")
            ycat = [sb(st, "ycat%d" % i, [128, 8, L], BF16) for i in range(2)]
            b_ycat = [Buf("ycat%d" % i) for i in range(2)]
            xts = [sb(st, "xt%d" % i, [128, D], F32) for i in range(2)]
            b_xts = [Buf("xt%d" % i) for i in range(2)]
            x1 = [sb(st, "x1%d" % i, [128, D], F32) for i in range(4)]
            b_x1 = [Buf("x1%d" % i) for i in range(4)]
            vf = [sb(st, "vf%d" % i, [128, D], F32) for i in range(4)]
            b_vf = [Buf("vf%d" % i) for i in range(4)]
            junkb = sb(st, "junkb", [128, D], BF16); b_junkb = Buf("junkb")
            vT = [sb(st, "vT%d" % i, [128, 8, 128], F32) for i in range(1)] * 2
            b_vT = [Buf("vT%d" % i) for i in range(1)] * 2
            dbg_x1 = ddbg("x1", [T, D]) if dbg else None

            def load_block(j, i):
                DMA(lambda: nc.sync.dma_start(
                    out=lxw[i][:], in_=lx_s.rearrange("c p t -> p c t")[:, :, 3 + j * L:3 + j * L + L + 3]),
                    r=[b_lx[j], b_lx[j + 1]], w=[b_lxw[i]])
                DMA(lambda: nc.sync.dma_start(
                    out=hfl[:], in_=hf_s.rearrange("c p t -> p c t")[:, :, j * L:(j + 1) * L]),
                    r=[b_hf[j]], w=[b_hfl])
                DMA(lambda: nc.sync.dma_start(
                    out=ggl[:], in_=glg_s.rearrange("c p t -> p c t")[:, :, j * L:(j + 1) * L]),
                    r=[b_glg[j]], w=[b_ggl])
                DMA(lambda: nc.sync.dma_start(
                    out=ycat[i][:, 0:4, :], in_=cx_s.rearrange("c p t -> p c t")[:, :, j * L:(j + 1) * L]),
                    r=[b_cx[j]], w=[b_ycat[i]])

            def merge(i, c):
                V(lambda: nc.vector.tensor_tensor(out=hh[i][:, c, :], in0=hh[i][:, c, :], in1=hfl[:, c, :],
                                                  op=ALU.add), r=[b_hh[i], b_hfl], w=[b_hh[i]])
                G(lambda: nc.gpsimd.tensor_tensor(out=ycat[i][:, 4 + c, :], in0=hh[i][:, c, :],
                                                  in1=ggl[:, c, :], op=ALU.mult),
                  r=[b_hh[i], b_ggl], w=[b_ycat[i]])

            def tile_s1(j, i, tt):
                g = j * 4 + tt
                xt, b_xt = xts[g % 2], b_xts[g % 2]
                xx, b_xx = x1[tt], b_x1[tt]
                DMA(lambda: nc.sync.dma_start(out=xt[:], in_=x_d[g * 128:(g + 1) * 128, :]), w=[b_xt])
                G(lambda: nc.gpsimd.tensor_tensor(out=xt[:], in0=xt[:], in1=bob[:], op=ALU.add),
                  r=[b_xt, b_bob], w=[b_xt])
                for n in range(2):
                    pb, b_pb = pbs[n]
                    for k in range(8):
                        P(lambda k=k, n=n, pb=pb: nc.tensor.matmul(
                            pb[:], lhsT=ycat[i][:, k, tt * 128:(tt + 1) * 128],
                            rhs=woutb[:, k, n * 512:(n + 1) * 512], start=(k == 0), stop=(k == 7)),
                          r=[b_ycat[i], b_woutb], w=[b_pb])
                    sl = slice(n * 512, (n + 1) * 512)
                    V(lambda pb=pb, sl=sl: nc.vector.tensor_tensor(out=xx[:, sl], in0=pb[:], in1=g1t[:, sl],
                                                                   op=ALU.mult), r=[b_pb, b_g1t], w=[b_xx])
                V(lambda: nc.vector.tensor_tensor(out=xx[:], in0=xx[:], in1=xt[:], op=ALU.add),
                  r=[b_xx, b_xt], w=[b_xx])
                if True:
                    DMA(lambda: nc.sync.dma_start(out=acc_s[g * 128:(g + 1) * 128, :], in_=xx[:]),
                        r=[b_xx], w=[b_acc])
                if dbg:
                    DMA(lambda: nc.sync.dma_start(out=dbg_x1[g * 128:(g + 1) * 128, :], in_=xx[:]), r=[b_xx])
                A(lambda: nc.scalar.activation(out=junkb[:], in_=xx[:], func=AF.Square,
                                               accum_out=ssq[:, g:g + 1]), r=[b_xx], w=[b_junkb, b_ssq])

            def rstd_block(j):
                g0 = j * 4
                A(lambda: nc.scalar.activation(out=rstd[:, g0:g0 + 4], in_=ssq[:, g0:g0 + 4], func=AF.Sqrt,
                                               scale=1.0 / D, bias=epsr[:, 0:1]), r=[b_ssq, b_eps], w=[b_rstd])
                V(lambda: nc.vector.reciprocal(out=rstd[:, g0:g0 + 4], in_=rstd[:, g0:g0 + 4]),
                  r=[b_rstd], w=[b_rstd])

            def tile_s2(j, tt):
                tile_s2a(j, tt)
                tile_s2b(j, tt)

            def tile_s2a(j, tt):
                g = j * 4 + tt
                xx, b_xx = x1[tt], b_x1[tt]
                v, b_v = vf[tt], b_vf[tt]
                V(lambda: nc.vector.scalar_tensor_tensor(out=v[:], in0=xx[:], scalar=rstd[:, g:g + 1],
                                                         in1=scb[:], op0=ALU.mult, op1=ALU.mult),
                  r=[b_xx, b_rstd, b_scb], w=[b_v])
                V(lambda: nc.vector.tensor_tensor(out=v[:], in0=v[:], in1=shb[:], op=ALU.add),
                  r=[b_v, b_shb], w=[b_v])
                if True:
                    vr, b_vr = vrow[g % 2], b_vrow[g % 2]
                    G(lambda: nc.gpsimd.iota(vr[:, 512:513], pattern=[[0, 1]], base=g * 128,
                                             channel_multiplier=1), w=[b_vr])
                    A(lambda: nc.scalar.copy(out=vr[:, 0:512].bitcast(BF16), in_=v[:]), r=[b_v], w=[b_vr])
                    DMA(lambda: nc.sync.dma_start(out=v_s[g // 4][(g % 4) * 128:(g % 4 + 1) * 128, :], in_=vr[:]),
                        r=[b_vr], w=[b_vs[g // 4]])

            def tile_s2b(j, tt):
                g = j * 4 + tt
                v, b_v = vf[tt], b_vf[tt]
                vt_, b_vt_ = vT[g % 2], b_vT[g % 2]
                for k in range(8):
                    P(lambda k=k: nc.tensor.transpose(out=pvT[:, k, :], in_=v[:, k * 128:(k + 1) * 128],
                                                      identity=identf[:]), r=[b_v, b_identf], w=[b_pvT])
                A(lambda: nc.scalar.copy(out=vt_[:], in_=pvT[:]), r=[b_pvT], w=[b_vt_])
                for k in range(8):
                    P(lambda k=k: nc.tensor.matmul(plg[:], lhsT=vt_[:, k, :], rhs=rwt[:, k, :],
                                                   start=(k == 0), stop=(k == 7)), r=[b_vt_, b_rwt], w=[b_plg])
                A(lambda: nc.scalar.copy(out=logit[:, g, :], in_=plg[:]), r=[b_plg], w=[b_logit])

            load_block(NBO - 1, 0)
            lr0 = Lru(lt, lxw[0], b_lxw[0], L, 1, hh[0], b_hh[0], pxs, b_pxs)
            lr0.run()
            for c in range(4):
                merge(0, c)
            for jj in range(NBO):
                j = NBO - 1 - jj
                i = jj % 2
                nxt = None
                if jj + 1 < NBO:
                    i2 = (jj + 1) % 2
                    load_block(j - 1, i2)
                    nxt = Lru(lt, lxw[i2], b_lxw[i2], L, 1, hh[i2], b_hh[i2], pxs, b_pxs)
                for tt in range(4):
                    if nxt is not None:
                        nxt.stageA1(tt)
                    tile_s1(j, i, tt)
                    if nxt is not None:
                        nxt.stageA2(tt)
                if nxt is not None:
                    nxt.stageB()
                rstd_block(j)
                for tt in range(4):
                    tile_s2a(j, tt)
                    if nxt is not None:
                        nxt.stageC(tt)
                        merge((jj + 1) % 2, tt)
                for tt in range(4):
                    tile_s2b(j, tt)
                S.coll(lambda j=j: nc.gpsimd.collective_compute(
                    "AllGather", ALU.bypass, replica_groups=groups,
                    ins=[v_s[j].opt()], outs=[ccvd[j].ap().opt()]),
                    reads=[b_vs[j]], writes=[b_ccvd[j]])
            DMA(lambda: nc.sync.dma_start(out=ccLs.ap()[:, :], in_=logit[:, 0:32, :].rearrange("p g e -> p (g e)")),
                r=[b_logit], w=[b_ccLs])
            S.coll(lambda: nc.gpsimd.collective_compute("AllGather", ALU.bypass, replica_groups=groups,
                                                        ins=[ccLs.ap().opt()], outs=[ccLd.ap().opt()]),
                   reads=[b_ccLs], writes=[b_ccLd])
            S.barrier()
            ck(4)

        lru_st.close()
        with ExitStack() as st:
            aff = sb(st, "aff", [128, 64, NE], F32); b_aff = Buf("aff")
            mx = sb(st, "mx", [128, 64], F32); b_mx = Buf("mx")
            affT = sb(st, "affT", [NE, T], F32); b_affT = Buf("affT")
            junk = sb(st, "junk", [NE, T], F32); b_junk = Buf("junk")
            pa = psum(st, "pa", [NE, 2048], F32); b_pa = Buf("pa")
            lgp = sb(st, "lgp", [128, 32, NE], F32); b_lgp = Buf("lgp")
            DMA(lambda: nc.gpsimd.indirect_dma_start(
                out=lgp[:].rearrange("p g e -> p (g e)"), out_offset=None, in_=ccLd.ap()[:, :],
                in_offset=bass.IndirectOffsetOnAxis(ap=pidx_t[:, 36:37], axis=0),
                bounds_check=reg_c1, oob_is_err=False), r=[b_pidxt, b_ccLd], w=[b_lgp], q="pool")
            vts = [sb(st, "mvts%d" % i, [128, 513], I32) for i in range(4)]
            b_vts = [Buf("vts%d" % i) for i in range(4)]
            for gp in range(32):
                vt, b_vt = vts[gp % 4], b_vts[gp % 4]
                DMA(lambda gp=gp, vt=vt: nc.gpsimd.indirect_dma_start(
                    out=vt[:, :], out_offset=None, in_=ccvd[gp // 4].ap()[:, :],
                    in_offset=bass.IndirectOffsetOnAxis(ap=pidx_t[:, gp % 4:gp % 4 + 1], axis=0),
                    bounds_check=reg_cc, oob_is_err=False),
                    r=[b_pidxt, b_ccvd[gp // 4]], w=[b_vt], q="pool")
                G(lambda vt=vt: nc.gpsimd.tensor_scalar(out=vt[:, 512:513], in0=vt[:, 512:513],
                                                        scalar1=float(TO), scalar2=None, op0=ALU.add),
                  r=[b_vt], w=[b_vt])
                DMA(lambda gp=gp, vt=vt: nc.sync.dma_start(out=vp_s[gp * 128:(gp + 1) * 128, :], in_=vt[:]),
                    r=[b_vt], w=[b_vp[gp]])
            V(lambda: nc.vector.tensor_copy(out=logit[:, 32:64, 0:NEL], in_=lgp[:, :, NEL:NE]), r=[b_lgp], w=[b_logit])
            V(lambda: nc.vector.tensor_copy(out=logit[:, 32:64, NEL:NE], in_=lgp[:, :, 0:NEL]), r=[b_lgp], w=[b_logit])
            V(lambda: nc.vector.tensor_reduce(out=mx[:], in_=logit[:], axis=AX.X, op=ALU.max), r=[b_logit], w=[b_mx])
            V(lambda: nc.vector.tensor_tensor(out=aff[:], in0=logit[:], in1=mx[:].unsqueeze(2).to_broadcast([128, 64, NE]),
                                              op=ALU.subtract), r=[b_logit, b_mx], w=[b_aff])
            A(lambda: nc.scalar.activation(out=aff[:], in_=aff[:], func=AF.Exp), r=[b_aff], w=[b_aff])
            V(lambda: nc.vector.tensor_reduce(out=mx[:], in_=aff[:], axis=AX.X, op=ALU.add), r=[b_aff], w=[b_mx])
            V(lambda: nc.vector.reciprocal(out=mx[:], in_=mx[:]), r=[b_mx], w=[b_mx])
            V(lambda: nc.vector.tensor_tensor(out=aff[:], in0=aff[:], in1=mx[:].unsqueeze(2).to_broadcast([128, 64, NE]),
                                              op=ALU.mult), r=[b_aff, b_mx], w=[b_aff])
            DMA(lambda: nc.sync.dma_start(out=aff_s[0:T, :].rearrange("(g p) e -> p g e", p=128), in_=aff[:, :, :]),
                r=[b_aff], w=[b_affs])
            for q in range(4):
                for g in range(16):
                    P(lambda q=q, g=g: nc.tensor.transpose(out=pa[:, g * 128:(g + 1) * 128], in_=aff[:, q * 16 + g, :],
                                                           identity=identf[:]), r=[b_aff, b_identf], w=[b_pa])
                A(lambda q=q: nc.scalar.copy(out=affT[:, q * 2048:(q + 1) * 2048], in_=pa[:]), r=[b_pa], w=[b_affT])
            thr = sb(st, "thr", [NE, 1], F32); b_thr = Buf("thr")
            thb = sb(st, "thb", [128, NEL], F32); b_thb = Buf("thb")
            cdb = sb(st, "cdb", [128, NEL], F32); b_cdb = Buf("cdb")
            cpb = sb(st, "cpb", [128, NEL], F32); b_cpb = Buf("cpb")
            msk = sb(st, "msk", [128, 64, NEL], F32); b_msk = Buf("msk")
            ones1 = sb(st, "ones1", [128, 128], F32); b_ones1 = Buf("ones1")
            pct = psum(st, "pct", [128, NEL], F32); b_pct = Buf("pct")
            V(lambda: nc.vector.memset(ones1[:], 1.0), w=[b_ones1])
            V(lambda: nc.vector.memset(thb[:], 0.0), w=[b_thb])
            for it in range(1, 25):
                step = 2.0 ** (-it)
                V(lambda step=step: nc.vector.tensor_scalar(out=cdb[:], in0=thb[:], scalar1=step, scalar2=None,
                                                            op0=ALU.add), r=[b_thb], w=[b_cdb])
                V(lambda: nc.vector.tensor_tensor(out=msk[:], in0=aff[:, :, 0:NEL],
                                                  in1=cdb[:].unsqueeze(1).to_broadcast([128, 64, NEL]), op=ALU.is_ge),
                  r=[b_aff, b_cdb], w=[b_msk])
                V(lambda: nc.vector.tensor_reduce(out=cpb[:], in_=msk[:].rearrange("p g e -> p e g"), axis=AX.X,
                                                  op=ALU.add), r=[b_msk], w=[b_cpb])
                P(lambda: nc.tensor.matmul(pct[:], lhsT=ones1[:], rhs=cpb[:], start=True, stop=True),
                  r=[b_ones1, b_cpb], w=[b_pct])
                V(lambda step=step: nc.vector.tensor_scalar(out=cpb[:], in0=pct[:], scalar1=float(CAP) - 0.5,
                                                            scalar2=step, op0=ALU.is_ge, op1=ALU.mult),
                  r=[b_pct], w=[b_cpb])
                V(lambda: nc.vector.tensor_tensor(out=thb[:], in0=thb[:], in1=cpb[:], op=ALU.add),
                  r=[b_thb, b_cpb], w=[b_thb])
            pth = psum(st, "pth", [NEL, 128], F32); b_pth = Buf("pth")
            V(lambda: nc.vector.memset(thr[:], 2.0), w=[b_thr])
            P(lambda: nc.tensor.transpose(out=pth[:], in_=thb[:], identity=identf[:]), r=[b_thb, b_identf], w=[b_pth])
            V(lambda: nc.vector.tensor_copy(out=thr[0:NEL, :], in_=pth[:, 0:1]), r=[b_pth], w=[b_thr])
            if dbg:
                dd = ddbg("thr", [NE, 1])
                DMA(lambda: nc.sync.dma_start(out=dd[:, :], in_=thr[:]), r=[b_thr])
                dd2 = ddbg("aff", [128, 64, NE])
                DMA(lambda: nc.sync.dma_start(out=dd2[:, :, :], in_=aff[:]), r=[b_aff])
            mk = junk[:, :]
            pst = sb(st, "pst", [NE, T], F32)
            ps_ = pst[:, :]
            zr = sb(st, "zr", [NE, T], F32); b_zr = Buf("zr")
            V(lambda: nc.vector.memset(zr[:], 0.0), w=[b_zr])
            V(lambda: nc.vector.tensor_scalar(out=mk, in0=affT[:, :], scalar1=thr[:, 0:1], scalar2=None,
                                              op0=ALU.is_ge), r=[b_affT, b_thr], w=[b_junk])
            V(lambda: nc.vector.tensor_tensor_scan(out=ps_, data0=mk, data1=zr[:], initial=0.0, op0=ALU.add,
                                                   op1=ALU.add), r=[b_junk, b_zr], w=[b_junk])
            V(lambda: nc.vector.tensor_scalar(out=mk, in0=mk, scalar1=-float(T), scalar2=float(T) - 1.0,
                                              op0=ALU.mult, op1=ALU.add), r=[b_junk], w=[b_junk])
            V(lambda: nc.vector.tensor_tensor(out=ps_, in0=ps_, in1=mk, op=ALU.add), r=[b_junk], w=[b_junk])
            pidx = psum(st, "pidx", [128, NT, NE], F32); b_pidx = Buf("pidx")
            for g in range(NT):
                P(lambda g=g: nc.tensor.transpose(out=pidx[:, g, :], in_=pst[:, g * 128:(g + 1) * 128],
                                                  identity=identf[0:NE, 0:NE]), r=[b_junk, b_identf], w=[b_pidx])
            V(lambda: nc.vector.tensor_copy(out=idx[:], in_=pidx[:]), r=[b_pidx], w=[b_idx])
            if dbg:
                dd = ddbg("idx", [128, NT, NE], I32)
                DMA(lambda: nc.sync.dma_start(out=dd[:, :, :], in_=idx[:]), r=[b_idx])
            S.barrier()
            ck(5)

        reg_cap = nc.gpsimd.to_reg(CAP - 1)
        reg_tot = nc.gpsimd.to_reg(T + TRASH - 1)
        with ExitStack() as st:
            xe = [sb(st, "xe%d" % i, [128, 4, 513], I32) for i in range(1)] * 2
            b_xet = [Buf("xet%d" % i) for i in range(1)] * 2
            toks = [sb(st, "toks%d" % i, [128, 8, 1], I32) for i in range(2)]
            b_toks = [Buf("toks%d" % i) for i in range(2)]
            m5b = sb(st, "m5b", [128, D], F32); b_m5b = Buf("m5b")
            gsel = [sb(st, "gsel%d" % i, [128, 8, NE], F32) for i in range(2)]
            b_gsel = [Buf("gsel%d" % i) for i in range(2)]
            xeT = sb(st, "xeT", [128, 8, CAP], BF16); b_xeT = Buf("xeT")
            hT = sb(st, "hT", [128, NF, CAP], BF16); b_hT = [Buf("hT%d" % i) for i in range(2)]
            wgf = [sb(st, "wgf%d" % i, [128, 8, 128], F32) for i in range(2)]
            b_wgf = [Buf("wgf%d" % i) for i in range(2)]
            wuf = [sb(st, "wuf%d" % i, [128, 8, 128], F32) for i in range(2)]
            b_wuf = [Buf("wuf%d" % i) for i in range(2)]
            wgb = [sb(st, "wgb%d" % i, [128, 8, 128], BF16) for i in range(2)]
            b_wgb = [Buf("wgb%d" % i) for i in range(2)]
            wub = [sb(st, "wub%d" % i, [128, 8, 128], BF16) for i in range(2)]
            b_wub = [Buf("wub%d" % i) for i in range(2)]
            wdf = [sb(st, "wdf%d" % i, [128, D], F32) for i in range(2)]
            b_wdf = [Buf("wdf%d" % i) for i in range(2)]
            wdb = sb(st, "wdb", [128, NF, D], BF16); b_wdb = Buf("wdb")
            hg = [sb(st, "hg%d" % i, [128, 512], F32) for i in range(2)]
            b_hg = [Buf("hg%d" % i) for i in range(2)]
            yst = [sb(st, "yst%d" % i, [128, D], F32) for i in range(1)] * 2
            b_yst = [Buf("yst%d" % i) for i in range(1)] * 2
            pgu = [(psum(st, "pg%d" % i, [128, 512], F32), Buf("pg%d" % i),
                    psum(st, "pu%d" % i, [128, 512], F32), Buf("pu%d" % i)) for i in range(2)]
            pdn = [(psum(st, "pd%d" % i, [128, 512], F32), Buf("pd%d" % i)) for i in range(3)]
            pxT = psum(st, "pxT", [128, 8, 128], BF16); b_pxT = Buf("pxT")
            make_bc(pdn[0:2], m5b, b_m5b, 0, 5 * D, "copy")
            NVT = 8
            vts = [sb(st, "vts%d" % i, [128, 513], I32) for i in range(NVT)]
            b_vts = [Buf("vts%d" % i) for i in range(NVT)]
            sc_rr = [0]

            def v_load(g, slot, q):
                vt, b_vt = vts[slot], b_vts[slot]
                eng = {"sp": nc.sync, "pool": nc.gpsimd, "act": nc.scalar}[q]
                if g < 32:
                    DMA(lambda: eng.dma_start(out=vt[:], in_=v_s[g // 4][(g % 4) * 128:(g % 4 + 1) * 128, :]),
                        r=[b_vs[g // 4]], w=[b_vt], q=q)
                else:
                    DMA(lambda: eng.dma_start(out=vt[:], in_=vp_s[(g - 32) * 128:(g - 31) * 128, :]),
                        r=[b_vp[g - 32]], w=[b_vt], q=q)

            def v_scatter(e, g, slot):
                vt, b_vt = vts[slot], b_vts[slot]
                DMA(lambda: nc.gpsimd.indirect_dma_start(
                    out=xe_s[e][:, :], out_offset=bass.IndirectOffsetOnAxis(ap=idx[:, g, e:e + 1], axis=0),
                    in_=vt[:, :], in_offset=None, bounds_check=reg_cap, oob_is_err=False),
                    r=[b_idx, b_vt, b_xe[e]], w=[b_xe[e]], q="pool")

            def scatter_stream(e, q):
                DEPTH = 4
                base = sc_rr[0]
                for g in range(DEPTH):
                    v_load(g, (base + g) % NVT, q)
                for g in range(NT):
                    v_scatter(e, g, (base + g) % NVT)
                    if e == 0:
                        v_scatter(1, g, (base + g) % NVT)
                    if g + DEPTH < NT:
                        v_load(g + DEPTH, (base + g + DEPTH) % NVT, q)
                sc_rr[0] = (base + NT) % NVT

            scatter_stream(0, "sp")
            cast_rr = 0
            gu_rr = 0
            dn_rr = 0
            for e in range(NEL):
                xi = e % 2
                xet, b_x = xe[xi], b_xet[xi]
                tk, b_tk = toks[xi], b_toks[xi]
                for hf_ in range(2):
                    DMA(lambda e=e, xet=xet, hf_=hf_: nc.sync.dma_start(
                        out=xet[:], in_=xe_s[e].rearrange("(b p) w -> p b w", p=128)[:, hf_ * 4:(hf_ + 1) * 4, :]),
                        r=[b_xe[e]], w=[b_x])
                    V(lambda xet=xet, tk=tk, hf_=hf_: nc.vector.tensor_copy(out=tk[:, hf_ * 4:(hf_ + 1) * 4, :],
                                                                           in_=xet[:, :, 512:513]),
                      r=[b_x], w=[b_tk])
                    for bl in range(4):
                        blk = hf_ * 4 + bl
                        for k in range(8):
                            P(lambda bl=bl, k=k, xet=xet: nc.tensor.transpose(
                                out=pxT[:, k, :], in_=xet[:, bl, 0:512].bitcast(BF16)[:, k * 128:(k + 1) * 128],
                                identity=ident[:]), r=[b_x, b_ident], w=[b_pxT])
                        if blk % 2 == 0:
                            A(lambda blk=blk: nc.scalar.copy(out=xeT[:, :, blk * 128:(blk + 1) * 128], in_=pxT[:]),
                              r=[b_pxT], w=[b_xeT])
                        else:
                            V(lambda blk=blk: nc.vector.tensor_copy(out=xeT[:, :, blk * 128:(blk + 1) * 128],
                                                                    in_=pxT[:]), r=[b_pxT], w=[b_xeT])
                for blk in range(8):
                    DMA(lambda blk=blk, tk=tk, xi=xi: nc.gpsimd.indirect_dma_start(
                        out=gsel[xi][:, blk, :], out_offset=None, in_=aff_s[:, :],
                        in_offset=bass.IndirectOffsetOnAxis(ap=tk[:, blk, :], axis=0),
                        bounds_check=reg_tot, oob_is_err=False),
                        r=[b_tk, b_affs], w=[b_gsel[xi]], q="pool")
                for f in range(NF):
                    wi = f % 2
                    DMA(lambda e=e, f=f, wi=wi: nc.sync.dma_start(
                        out=wgf[wi][:], in_=wg_d[e, f]),
                        w=[b_wgf[wi]])
                    DMA(lambda e=e, f=f, wi=wi: nc.sync.dma_start(
                        out=wuf[wi][:], in_=wu_d[e, f]),
                        w=[b_wuf[wi]])
                    A(lambda wi=wi: nc.scalar.copy(out=wgb[wi][:], in_=wgf[wi][:]), r=[b_wgf[wi]], w=[b_wgb[wi]])
                    if f % 4 < 2:
                        V(lambda wi=wi: nc.vector.tensor_copy(out=wub[wi][:], in_=wuf[wi][:]),
                          r=[b_wuf[wi]], w=[b_wub[wi]])
                    else:
                        A(lambda wi=wi: nc.scalar.copy(out=wub[wi][:], in_=wuf[wi][:]),
                          r=[b_wuf[wi]], w=[b_wub[wi]])
                    for ns in range(2):
                        pg, b_pg, pu, b_pu = pgu[gu_rr % 2]
                        hgi, b_hgi = hg[gu_rr % 2], b_hg[gu_rr % 2]
                        gu_rr += 1
                        for k in range(8):
                            P(lambda k=k, wi=wi, ns=ns, pg=pg: nc.tensor.matmul(
                                pg[:], lhsT=wgb[wi][:, k, :],
                                rhs=xeT[:, k, ns * 512:(ns + 1) * 512], start=(k == 0), stop=(k == 7)),
                              r=[b_wgb[wi], b_xeT], w=[b_pg])
                        for k in range(8):
                            P(lambda k=k, wi=wi, ns=ns, pu=pu: nc.tensor.matmul(
                                pu[:], lhsT=wub[wi][:, k, :],
                                rhs=xeT[:, k, ns * 512:(ns + 1) * 512], start=(k == 0), stop=(k == 7)),
                              r=[b_wub[wi], b_xeT], w=[b_pu])
                        A(lambda pg=pg, hgi=hgi: nc.scalar.activation(out=hgi[:], in_=pg[:], func=AF.Silu),
                          r=[b_pg], w=[b_hgi])
                        V(lambda pu=pu, hgi=hgi, f=f, ns=ns: nc.vector.tensor_tensor(
                            out=hT[:, f, ns * 512:(ns + 1) * 512], in0=pu[:], in1=hgi[:], op=ALU.mult),
                          r=[b_pu, b_hgi], w=[b_hT[ns]])
                    DMA(lambda e=e, f=f, wi=wi: nc.sync.dma_start(
                        out=wdf[wi][:], in_=wd_d[e, f * 128:(f + 1) * 128, :]), w=[b_wdf[wi]])
                    V(lambda f=f, wi=wi: nc.vector.tensor_copy(out=wdb[:, f, :], in_=wdf[wi][:]),
                      r=[b_wdf[wi]], w=[b_wdb])
                    if e % 2 == 1 and e + 1 < NEL:
                        for g in range(f * 3, min(NT, f * 3 + 3)):
                            slot = sc_rr[0] % NVT
                            sc_rr[0] += 1
                            v_load(g, slot, "act")
                            v_scatter(e + 1, g, slot)
                            if e + 2 < NEL:
                                v_scatter(e + 2, g, slot)

                for blk in range(8):
                    yi = blk % 2
                    ys, b_ys = yst[yi], b_yst[yi]
                    for n in range(2):
                        pd, b_pd = pdn[dn_rr % 3]
                        dn_rr += 1
                        for f in range(NF):
                            P(lambda f=f, blk=blk, n=n, pd=pd: nc.tensor.matmul(
                                pd[:], lhsT=hT[:, f, blk * 128:(blk + 1) * 128], rhs=wdb[:, f, n * 512:(n + 1) * 512],
                                start=(f == 0), stop=(f == NF - 1)),
                              r=[b_hT[blk // 4], b_wdb], w=[b_pd])
                        sl = slice(n * 512, (n + 1) * 512)
                        V(lambda pd=pd, ys=ys, sl=sl, blk=blk, xi=xi, e=e: nc.vector.scalar_tensor_tensor(
                            out=ys[:, sl], in0=pd[:], scalar=gsel[xi][:, blk, e:e + 1], in1=m5b[:, sl],
                            op0=ALU.mult, op1=ALU.mult), r=[b_pd, b_gsel[xi], b_m5b], w=[b_ys])
                    DMA(lambda blk=blk, ys=ys, tk=tk: nc.gpsimd.indirect_dma_start(
                        out=acc_s[:, :], out_offset=bass.IndirectOffsetOnAxis(ap=tk[:, blk, :], axis=0),
                        in_=ys[:], in_offset=None, bounds_check=reg_tot, oob_is_err=True,
                        compute_op=ALU.add), r=[b_ys, b_tk, b_acc], w=[b_acc], q="pool")
            S.barrier()

        for k in range(NCC):
            DMA(lambda k=k: nc.sync.dma_start(
                out=ccsrc[k].ap().rearrange("p (q n) -> p q n", q=4),
                in_=acc_s[TO + k * CCR:TO + (k + 1) * CCR, :].rearrange("(q p) n -> p q n", p=128)),
                r=[b_acc], w=[b_ccsrc[k]])
        for k in range(NCC):
            S.coll(lambda k=k: nc.gpsimd.collective_compute(
                "AllGather", ALU.bypass, replica_groups=groups,
                ins=[ccsrc[k].ap().opt()], outs=[ccdst[k].ap().opt()]),
                reads=[b_ccsrc[k]], writes=[b_ccdst[k]])

        with ExitStack() as st:
            rp3, b_rp3 = load_rowp(st, 3)
            xt4 = [sb(st, "fx%d" % i, [128, 4, D], F32) for i in range(2)]
            b_xt4 = [Buf("fx%d" % i) for i in range(2)]
            pr4 = [sb(st, "fp%d" % i, [128, 4, D], F32) for i in range(2)]
            b_pr4 = [Buf("fp%d" % i) for i in range(2)]
            ot4 = [sb(st, "fo%d" % i, [128, 4, D], F32) for i in range(2)]
            b_ot4 = [Buf("fo%d" % i) for i in range(2)]
            tmp = sb(st, "ftmp", [128, D], BF16); b_tmp = Buf("ftmp")
            b_out = Buf("out")
            for k in range(NCC):
                i = k % 2
                DMA(lambda k=k, i=i: nc.sync.dma_start(
                    out=xt4[i][:], in_=acc_s[k * CCR:(k + 1) * CCR, :].rearrange("(q p) n -> p q n", p=128)),
                    r=[b_acc], w=[b_xt4[i]])
                DMA(lambda k=k, i=i: nc.gpsimd.indirect_dma_start(
                    out=pr4[i][:].rearrange("p q n -> p (q n)"), out_offset=None, in_=ccdst[k].ap()[:, :],
                    in_offset=bass.IndirectOffsetOnAxis(ap=pidx_t[:, 36:37], axis=0),
                    bounds_check=reg_c1, oob_is_err=False),
                    r=[b_pidxt, b_ccdst[k]], w=[b_pr4[i]], q="pool")
                V(lambda i=i: nc.vector.tensor_tensor(out=xt4[i][:], in0=xt4[i][:], in1=pr4[i][:], op=ALU.add),
                  r=[b_xt4[i], b_pr4[i]], w=[b_xt4[i]])
                for q_ in range(4):
                    g = k * 4 + q_
                    A(lambda i=i, q_=q_, g=g: nc.scalar.activation(out=tmp[:], in_=xt4[i][:, q_, :], func=AF.Square,
                                                                   accum_out=ssq[:, g:g + 1]),
                      r=[b_xt4[i]], w=[b_tmp, b_ssq])
                g0 = k * 4
                A(lambda g0=g0: nc.scalar.activation(out=rstd[:, g0:g0 + 4], in_=ssq[:, g0:g0 + 4], func=AF.Sqrt,
                                                     scale=1.0 / D, bias=epsr[:, 0:1]), r=[b_ssq, b_eps], w=[b_rstd])
                V(lambda g0=g0: nc.vector.reciprocal(out=rstd[:, g0:g0 + 4], in_=rstd[:, g0:g0 + 4]),
                  r=[b_rstd], w=[b_rstd])
                for q_ in range(4):
                    g = k * 4 + q_
                    V(lambda i=i, q_=q_, g=g: nc.vector.scalar_tensor_tensor(
                        out=ot4[i][:, q_, :], in0=xt4[i][:, q_, :], scalar=rstd[:, g:g + 1], in1=rp3[:],
                        op0=ALU.mult, op1=ALU.mult), r=[b_xt4[i], b_rstd, b_rp3], w=[b_ot4[i]])
                DMA(lambda k=k, i=i: nc.sync.dma_start(
                    out=out_d[k * CCR:(k + 1) * CCR, :].rearrange("(q p) n -> p q n", p=128), in_=ot4[i][:]),
                    r=[b_ot4[i]], w=[b_out])
            S.barrier()
        big.close()
        print("kernel build: insts", S.n_inst, "waits", S.n_wait)
    except _Stop:
        pass
    return nc, dbg_out


_WCACHE = {}


def _expert_layout(inp, grp):
    key = (id(inp["exp_w_gate"]), grp)
    if key not in _WCACHE:
        es_ = slice(grp * NEL, (grp + 1) * NEL)
        out = []
        for name in ("exp_w_gate", "exp_w_up"):
            w = inp[name][0][es_]
            w = w.reshape(NEL, 8, 128, NF, 128).transpose(0, 3, 2, 1, 4)
            out.append(np.ascontiguousarray(w, dtype=np.float32))
        _WCACHE[key] = out
    return _WCACHE[key]


def _prep_core(c, inp):
    b, flip = c // 2, c % 2
    f32 = np.float32
    xb = inp["x"][b]
    ctxb = inp["ctx"][b]
    conv_w = inp["conv_dw_w"][0]
    if flip:
        xb = xb[::-1]
        ctxb = ctxb[::-1]
        conv_w = conv_w[::-1]
    dirs = [1, 0] if flip else [0, 1]
    cc = np.zeros((128, 8, 2), f32)
    cc[:, :, 0] = inp["c"][b].reshape(8, 128).T
    cc[:, :, 1] = inp["c_ctx"].reshape(8, 128).T
    rowp = np.zeros((128, 4, D), f32)
    rowp[:, 0, :] = inp["norm1_g"][0][None, :]
    rowp[:, 1, :] = inp["norm2_g"][0][None, :]
    rowp[:, 2, :] = inp["b_out"][0][None, :]
    rowp[:, 3, :] = inp["final_norm_g"][None, :]
    colp = np.zeros((128, NCOL), f32)
    colp[:, C_BIN:C_BIN + 16] = inp["b_in"][0].reshape(16, 128).T
    colp[:, C_CB:C_CB + 4] = inp["conv_dw_b"][0].reshape(4, 128).T
    colp[:, C_LG:C_LG + 4] = inp["conv_ln_g"][0].reshape(4, 128).T
    colp[:, C_LB:C_LB + 4] = inp["conv_ln_b"][0].reshape(4, 128).T
    colp[:, C_CW:C_CW + 124] = conv_w.reshape(31, 4, 128).transpose(2, 1, 0).reshape(128, 124)
    gw = np.zeros((128, 2, 2, 4, 128), f32)
    for dp, d in enumerate(dirs):
        colp[:, C_LCB + dp * 4:C_LCB + dp * 4 + 4] = inp["lru_conv_b"][0, d].reshape(4, 128).T
        colp[:, C_BA + dp * 4:C_BA + dp * 4 + 4] = inp["lru_ba"][0, d].reshape(4, 128).T
        colp[:, C_BI + dp * 4:C_BI + dp * 4 + 4] = inp["lru_bi"][0, d].reshape(4, 128).T
        colp[:, C_LAM + dp * 4:C_LAM + dp * 4 + 4] = inp["lru_lambda"][0, d].reshape(4, 128).T
        colp[:, C_LCW + dp * 16:C_LCW + dp * 16 + 16] = \
            inp["lru_conv_w"][0, d].reshape(4, 4, 128).transpose(2, 1, 0).reshape(128, 16)
        for gi, key in enumerate(("lru_wa", "lru_wi")):
            w = inp[key][0, d]
            for cch in range(4):
                for hh in range(2):
                    gw[hh * 64:(hh + 1) * 64, dp, gi, cch, hh * 64:(hh + 1) * 64] = w[2 * cch + hh]
    grp = c % 2
    perm = list(range(grp * NEL, (grp + 1) * NEL)) + list(range((1 - grp) * NEL, (2 - grp) * NEL))
    rw = inp["router_w"][0][:, perm]
    pidx = np.zeros((128, 40), np.int32)
    rp = 1 - grp
    ar = np.arange(128)
    for q in range(4):
        pidx[:, q] = rp * CCR + q * 128 + ar
    for g in range(32):
        pidx[:, 4 + g] = rp * TO + g * 128 + ar
    pidx[:, 36] = rp * 128 + ar
    es_ = slice(grp * NEL, (grp + 1) * NEL)
    return {
        "pidx": pidx,
        "xb": np.ascontiguousarray(xb, dtype=f32),
        "ctxb": np.ascontiguousarray(ctxb, dtype=f32),
        "cc": cc,
        "ada_w": np.ascontiguousarray(inp["ada_w"][0], dtype=f32),
        "ada_b2": np.ascontiguousarray(np.broadcast_to(inp["ada_b"][0][None, :], (2, 6 * D)), dtype=f32),
        "rowp": rowp,
        "colp": colp,
        "w_in": np.ascontiguousarray(inp["w_in"][0], dtype=f32),
        "w_out": np.ascontiguousarray(inp["w_out"][0], dtype=f32),
        "router_w": np.ascontiguousarray(rw.reshape(8, 128, NE).transpose(1, 0, 2), dtype=f32),
        "gw": gw,
        "wg": _expert_layout(inp, grp)[0],
        "wu": _expert_layout(inp, grp)[1],
        "wd": np.ascontiguousarray(inp["exp_w_down"][0][es_], dtype=f32),
    }


def kernel(**inputs):
    inp = {k: np.asarray(v) for k, v in inputs.items()}
    nc, _ = build_nc(DEBUG)
    in_maps = [_prep_core(c, inp) for c in range(8)]
    res = run_bass_kernel_spmd(nc, in_maps, core_ids=list(range(8)))
    _WCACHE.clear()
    out = np.zeros((4, T, D), np.float32)
    for c in range(8):
        b, flip = c // 2, c % 2
        o = np.asarray(res.results[c]["out"], dtype=np.float32)
        if flip:
            out[b, TO:] = o[::-1]
        else:
            out[b, :TO] = o
    return out
```
